# Optimizing a Trainium2 kernel written in Bass

```python
import math
import jax, jax.numpy as jnp
from jax import lax
import numpy as np

D_MODEL = 1024
BATCH = 32
SEQ = 2048
DEPTH = 2

HEAD_DIM = 64
MEM_LEN = 256
NORM_EPS = 1e-6
NEG = -1e30
SCALE = HEAD_DIM ** -0.5
A_HEADS = 4
A_CONFIGS = ((128, 1), (512, 4), (2048, 16))
B_HEADS = 4
SB_BLOCK = 128
C_HEADS = 4
C_KV_HEADS = 2
C_WINDOW = 128
D_HEADS = 4
MOBA_BLOCK = 256
MOBA_TOPK = 3
MOBA_QCHUNK = 16
BAND_BLOCK = 128
REL_BUCKETS = 32
REL_MAX_DIST = 2048
A_BIAS_LO = 0
C_BIAS_LO = A_HEADS
D_BIAS_LO = A_HEADS + C_HEADS
REL_HEADS = A_HEADS + C_HEADS + D_HEADS
X_HEADS = 4
X_HEAD_DIM = 64
X_W = X_HEADS * X_HEAD_DIM
X_SCALE = X_HEAD_DIM ** -0.5
D_FF = 4 * D_MODEL
A_W = A_HEADS * HEAD_DIM
B_W = B_HEADS * HEAD_DIM
C_QW = C_HEADS * HEAD_DIM
C_KVW = C_KV_HEADS * HEAD_DIM
D_W = D_HEADS * HEAD_DIM
IN_WIDTH = 3 * A_W + 3 * B_W + C_QW + 2 * C_KVW + 3 * D_W
MIX_WIDTH = A_W + B_W + C_QW + D_W

kernel_name = "hybrid_dilated_stickbreak_swa_moba_block"


def rmsnorm(x, g):
    xf = x.astype(jnp.float32)
    y = xf * lax.rsqrt(jnp.mean(xf * xf, axis=-1, keepdims=True) + NORM_EPS)
    return (y * g.astype(jnp.float32)).astype(x.dtype)


def rel_bucket(dist):
    max_exact = REL_BUCKETS // 2
    n = jnp.maximum(dist, 0)
    nf = jnp.maximum(n, 1).astype(jnp.float32)
    large = max_exact + (jnp.log(nf / max_exact) / math.log(REL_MAX_DIST / max_exact)
                         * (REL_BUCKETS - max_exact)).astype(jnp.int32)
    large = jnp.minimum(large, REL_BUCKETS - 1)
    return jnp.where(n < max_exact, n, large)


def rel_bias_heads(rel_table, dist, head_lo, n_heads):
    tab = rel_table[:, head_lo:head_lo + n_heads].astype(jnp.float32)
    return jnp.moveaxis(tab[rel_bucket(dist)], -1, 0)


def pad_to_multiple(x, axis, mult):
    n = x.shape[axis]
    pad = (-n) % mult
    if pad == 0:
        return x
    widths = [(0, 0)] * x.ndim
    widths[axis] = (0, pad)
    return jnp.pad(x, widths)


def to_blocks(x, blk):
    return x.reshape(x.shape[:-2] + (x.shape[-2] // blk, blk, x.shape[-1]))


def band_keys(xb):
    prev = jnp.concatenate([jnp.zeros_like(xb[..., :1, :, :]), xb[..., :-1, :, :]], axis=-3)
    return jnp.concatenate([prev, xb], axis=-2)


def band_geometry(n_blocks):
    qi = jnp.arange(BAND_BLOCK)[:, None]
    ki = jnp.arange(2 * BAND_BLOCK)[None, :]
    dist = qi + BAND_BLOCK - ki
    first_ok = (jnp.arange(n_blocks)[:, None, None] > 0) | (ki >= BAND_BLOCK)[None]
    return dist, first_ok


def dilated_attention(q, k, v, rel_table):
    B, H, S, E = q.shape
    blk = BAND_BLOCK
    nums, maxs, dens = [], [], []
    for window, dil in A_CONFIGS:
        span = window // dil
        L = S // dil

        def residue_blocks(t):
            t = t.reshape(B, H, L, dil, E).transpose(0, 1, 3, 2, 4)
            return to_blocks(pad_to_multiple(t, 3, blk), blk)

        qb = residue_blocks(q)
        kb = band_keys(residue_blocks(k))
        vb = band_keys(residue_blocks(v))
        nb = qb.shape[3]
        dist, first_ok = band_geometry(nb)
        valid = (dist >= 0) & (dist <= span) & first_ok
        bias = rel_bias_heads(rel_table, dist * dil, A_BIAS_LO, H)
        s = jnp.einsum("bhrnqe,bhrnke->bhrnqk", qb, kb) * SCALE + bias[None, :, None, None]
        s = jnp.where(valid, s, NEG)
        m = jnp.max(s, axis=-1)
        p = jnp.exp(s - m[..., None])
        l = jnp.sum(p, axis=-1)
        o = jnp.einsum("bhrnqk,bhrnke->bhrnqe", p, vb)

        def unres(t):
            t = t.reshape((B, H, dil, nb * blk) + t.shape[5:])[:, :, :, :L]
            t = jnp.swapaxes(t, 2, 3)
            return t.reshape((B, H, S) + t.shape[4:])

        nums.append(unres(o))
        maxs.append(unres(m))
        dens.append(unres(l))
    mx = jnp.stack(maxs)
    w = jnp.exp(mx - jnp.max(mx, axis=0, keepdims=True))
    num = jnp.einsum("cbhs,cbhse->bhse", w, jnp.stack(nums))
    den = jnp.sum(w * jnp.stack(dens), axis=0)
    return num / den[..., None]


def stick_breaking_attention(q, k, v):
    B, H, S, E = q.shape
    nblk = S // SB_BLOCK
    qs = q.reshape(B, H, nblk, SB_BLOCK, E).transpose(2, 0, 1, 3, 4)
    kpos = jnp.arange(S)

    def one_block(args):
        qb, i = args
        qpos = i * SB_BLOCK + jnp.arange(SB_BLOCK)
        z = jnp.einsum("bhqe,bhke->bhqk", qb, k) * SCALE
        past = kpos[None, :] < qpos[:, None]
        log_1m = jnp.where(past, jax.nn.log_sigmoid(-z), 0.0)
        suffix = lax.cumsum(log_1m, axis=3, reverse=True) - log_1m
        a = jnp.where(past, jnp.exp(jax.nn.log_sigmoid(z) + suffix), 0.0)
        return jnp.einsum("bhqk,bhke->bhqe", a, v)

    out = lax.map(one_block, (qs, jnp.arange(nblk)))
    return out.transpose(1, 2, 0, 3, 4).reshape(B, H, S, E)


def sliding_window_sink_attention(q, k, v, sinks, rel_table):
    B, Hq, S, E = q.shape
    G = k.shape[1]
    R = Hq // G
    blk = BAND_BLOCK
    qb = to_blocks(q.reshape(B, G, R, S, E), blk)
    kb = band_keys(to_blocks(k, blk))
    vb = band_keys(to_blocks(v, blk))
    nb = qb.shape[3]
    dist, first_ok = band_geometry(nb)
    valid = (dist >= 0) & (dist < C_WINDOW) & first_ok
    bias = rel_bias_heads(rel_table, dist, C_BIAS_LO, Hq).reshape(G, R, blk, 2 * blk)
    s = jnp.einsum("bgrnqe,bgnke->bgrnqk", qb, kb) * SCALE + bias[None, :, :, None]
    s = jnp.where(valid, s, NEG)
    sink = sinks.astype(jnp.float32).reshape(G, R)[None, :, :, None, None, None]
    m = jnp.maximum(jnp.max(s, axis=-1, keepdims=True), sink)
    p = jnp.exp(s - m)
    den = jnp.sum(p, axis=-1, keepdims=True) + jnp.exp(sink - m)
    o = jnp.einsum("bgrnqk,bgnke->bgrnqe", p / den, vb)
    return o.reshape(B, Hq, S, E)


def moba_attention(q, k, v, rel_table):
    B, H, S, E = q.shape
    blk = MOBA_BLOCK
    qp = pad_to_multiple(q, 2, blk)
    kp = pad_to_multiple(k, 2, blk)
    vp = pad_to_multiple(v, 2, blk)
    Sp = qp.shape[2]
    nblk = Sp // blk
    kblk = kp.reshape(B, H, nblk, blk, E)
    vblk = vp.reshape(B, H, nblk, blk, E)
    kmean = jnp.mean(kblk, axis=3)
    qblock = jnp.arange(Sp) // blk
    fully_past = jnp.arange(nblk)[None, :] < qblock[:, None]
    gate = jnp.where(fully_past, jnp.einsum("bhse,bhne->bhsn", qp, kmean), NEG)
    n_sel = min(MOBA_TOPK, nblk)
    _, sel = lax.top_k(gate, n_sel)
    sel_ok = sel < qblock[:, None]
    tab = rel_table[:, D_BIAS_LO:D_BIAS_LO + H].T.astype(jnp.float32)
    b_ix = jnp.arange(B)[:, None, None, None]
    h_ix = jnp.arange(H)[None, :, None, None]
    offs = jnp.arange(blk)

    def one_chunk(c):
        start = c * MOBA_QCHUNK
        qc = lax.dynamic_slice_in_dim(qp, start, MOBA_QCHUNK, axis=2)
        idx = lax.dynamic_slice_in_dim(sel, start, MOBA_QCHUNK, axis=2)
        ok = lax.dynamic_slice_in_dim(sel_ok, start, MOBA_QCHUNK, axis=2)
        tpos = start + jnp.arange(MOBA_QCHUNK)
        own = start // blk
        k_own = lax.dynamic_index_in_dim(kblk, own, axis=2, keepdims=False)
        v_own = lax.dynamic_index_in_dim(vblk, own, axis=2, keepdims=False)
        d_own = tpos[:, None] - (own * blk + offs)[None, :]
        s_own = jnp.einsum("bhqe,bhke->bhqk", qc, k_own) * SCALE + tab[:, rel_bucket(d_own)][None]
        s_own = jnp.where(d_own >= 0, s_own, NEG)
        k_sel = kblk[b_ix, h_ix, idx]
        v_sel = vblk[b_ix, h_ix, idx]
        d_sel = tpos[:, None, None] - (idx[..., None] * blk + offs)
        s_sel = (jnp.einsum("bhqe,bhqjke->bhqjk", qc, k_sel) * SCALE
                 + tab[h_ix[..., None], rel_bucket(d_sel)])
        s_sel = jnp.where(ok[..., None], s_sel, NEG).reshape(B, H, MOBA_QCHUNK, n_sel * blk)
        p = jax.nn.softmax(jnp.concatenate([s_own, s_sel], axis=-1), axis=-1)
        p_own = p[..., :blk]
        p_sel = p[..., blk:].reshape(B, H, MOBA_QCHUNK, n_sel, blk)
        return (jnp.einsum("bhqk,bhke->bhqe", p_own, v_own)
                + jnp.einsum("bhqjk,bhqjke->bhqe", p_sel, v_sel))

    out = lax.map(one_chunk, jnp.arange(Sp // MOBA_QCHUNK))
    return out.transpose(1, 2, 0, 3, 4).reshape(B, H, Sp, E)[:, :, :S]


def hybrid_mixer(h, w_in, g_group, sinks, w_out, rel_table):
    B, S, _ = h.shape
    proj = (h @ w_in).astype(jnp.float32)
    sizes = (A_W, A_W, A_W, B_W, B_W, B_W, C_QW, C_KVW, C_KVW, D_W, D_W, D_W)
    cuts = []
    acc = 0
    for sz in sizes[:-1]:
        acc += sz
        cuts.append(acc)
    aq, ak, av, bq, bk, bv, cq, ck, cv, dq, dk, dv = jnp.split(proj, cuts, axis=-1)

    def heads(t):
        return t.reshape(B, S, -1, HEAD_DIM).transpose(0, 2, 1, 3)

    ya = dilated_attention(heads(aq), heads(ak), heads(av), rel_table)
    yb = stick_breaking_attention(heads(bq), heads(bk), heads(bv))
    yc = sliding_window_sink_attention(heads(cq), heads(ck), heads(cv), sinks, rel_table)
    yd = moba_attention(heads(dq), heads(dk), heads(dv), rel_table)
    groups = []
    for y in (ya, yb, yc, yd):
        y = y.transpose(0, 2, 1, 3).reshape(B, S, -1)
        groups.append(y * lax.rsqrt(jnp.mean(y * y, axis=-1, keepdims=True) + NORM_EPS))
    y = jnp.concatenate(groups, axis=-1) * g_group.astype(jnp.float32)
    return y.astype(h.dtype) @ w_out


def memory_cross_attention(h, m, w_q, w_kv, w_o):
    B, S, _ = h.shape
    M = m.shape[1]
    q = (h @ w_q).astype(jnp.float32).reshape(B, S, X_HEADS, X_HEAD_DIM)
    kv = (m @ w_kv).astype(jnp.float32)
    k = kv[..., :X_W].reshape(B, M, X_HEADS, X_HEAD_DIM)
    v = kv[..., X_W:].reshape(B, M, X_HEADS, X_HEAD_DIM)
    p = jax.nn.softmax(jnp.einsum("bshe,bmhe->bhsm", q, k) * X_SCALE, axis=-1)
    o = jnp.einsum("bhsm,bmhe->bshe", p, v).reshape(B, S, X_W)
    return o.astype(h.dtype) @ w_o


def squared_relu_mlp(h, w_up, w_down):
    return jnp.square(jax.nn.relu(h @ w_up)) @ w_down


def setup_inputs(seed: int = 0) -> dict:
    key = jax.random.key(seed)
    ks = jax.random.split(key, 17)
    f32 = jnp.float32

    def nrm(k, shape, scale):
        return jax.random.normal(k, shape, f32) * scale

    def gain(k, shape):
        return 1.0 + 0.02 * jax.random.normal(k, shape, f32)

    return {
        "x": nrm(ks[0], (BATCH, SEQ, D_MODEL), 1.0),
        "mem": nrm(ks[1], (BATCH, MEM_LEN, D_MODEL), 1.0),
        "rel_table": nrm(ks[2], (REL_BUCKETS, REL_HEADS), 0.5),
        "g_mix": gain(ks[3], (DEPTH, D_MODEL)),
        "w_in": nrm(ks[4], (DEPTH, D_MODEL, IN_WIDTH), D_MODEL ** -0.5),
        "g_group": gain(ks[5], (DEPTH, MIX_WIDTH)),
        "sinks": nrm(ks[6], (DEPTH, C_HEADS), 0.5),
        "w_out": nrm(ks[7], (DEPTH, MIX_WIDTH, D_MODEL), MIX_WIDTH ** -0.5),
        "g_cross": gain(ks[8], (DEPTH, D_MODEL)),
        "g_mem": gain(ks[9], (DEPTH, D_MODEL)),
        "w_xq": nrm(ks[10], (DEPTH, D_MODEL, X_W), D_MODEL ** -0.5),
        "w_xkv": nrm(ks[11], (DEPTH, D_MODEL, 2 * X_W), D_MODEL ** -0.5),
        "w_xo": nrm(ks[12], (DEPTH, X_W, D_MODEL), X_W ** -0.5),
        "g_mlp": gain(ks[13], (DEPTH, D_MODEL)),
        "w_up": nrm(ks[14], (DEPTH, D_MODEL, D_FF), D_MODEL ** -0.5),
        "w_down": nrm(ks[15], (DEPTH, D_FF, D_MODEL), D_FF ** -0.5),
        "g_final": gain(ks[16], (D_MODEL,)),
    }


def reference(x, mem, rel_table, g_mix, w_in, g_group, sinks, w_out, g_cross, g_mem,
              w_xq, w_xkv, w_xo, g_mlp, w_up, w_down, g_final):
    for l in range(DEPTH):
        x = x + hybrid_mixer(rmsnorm(x, g_mix[l]), w_in[l], g_group[l], sinks[l], w_out[l], rel_table)
        x = x + memory_cross_attention(rmsnorm(x, g_cross[l]), rmsnorm(mem, g_mem[l]),
                                       w_xq[l], w_xkv[l], w_xo[l])
        x = x + squared_relu_mlp(rmsnorm(x, g_mlp[l]), w_up[l], w_down[l])
    return rmsnorm(x, g_final)
```

```python
import math
import os as _os
import contextlib
import numpy as np
import ml_dtypes
import concourse.bass as bass
import concourse.mybir as mybir
from concourse.bass_utils import run_bass_kernel_spmd

F32 = mybir.dt.float32
BF16 = mybir.dt.bfloat16
AF = mybir.ActivationFunctionType
ALU = mybir.AluOpType

D = 1024
SEQ = 2048
NT = SEQ // 128
MEM = 256
DEPTH = 2
IN_W = 2816
DFF = 4096
NDIST = SEQ + 127
EPS = 1e-6
N_CORES = 8
BIG = 1.0e30

A_Q, A_K, A_V = 0, 256, 512
B_Q, B_K, B_V = 768, 1024, 1280
C_Q, C_K, C_V = 1536, 1792, 1920
D_Q, D_K, D_V = 2048, 2304, 2560


class Buf:
    __slots__ = ("name", "lw", "rd")

    def __init__(self, name):
        self.name = name
        self.lw = None
        self.rd = {}


class Tile:
    def __init__(self, t, b):
        self.t = t
        self.b = b


class Sched:
    EPOCH = 16000

    def __init__(self, nc, es):
        self.nc = nc
        self.es = es
        self.eng = {"pe": nc.tensor, "act": nc.scalar, "dve": nc.vector, "pool": nc.gpsimd, "sp": nc.sync}
        self.cnt = {k: 0 for k in self.eng}
        self.sems = {k: [] for k in self.eng}
        self.seen = {k: {} for k in self.eng}
        self.seen_dma = {k: set() for k in self.eng}
        self.q = {}
        self.rr = {}

    def newsem(self, name):
        return self.es.enter_context(self.nc.semaphore(name))

    def add_queue(self, name, issuer, K=8):
        self.q[name] = dict(issuer=issuer, sems=[self.newsem(f"dq_{name}_{i}") for i in range(K)], n=0, K=K)

    def _sem_for(self, e, c):
        idx = (c - 1) // self.EPOCH
        while len(self.sems[e]) <= idx:
            self.sems[e].append(self.newsem(f"s_{e}_{len(self.sems[e])}"))
        return self.sems[e][idx], c - idx * self.EPOCH

    def _wait(self, e, tok):
        if tok[0] == "e":
            _, x, c = tok
            if e == "pe" and x == "pe":
                return
            if self.seen[e].get(x, 0) >= c:
                return
            sem, v = self._sem_for(x, c)
            self.eng[e].wait_ge(sem, v)
            self.seen[e][x] = c
        else:
            _, qn, i = tok
            if tok in self.seen_dma[e]:
                return
            Q = self.q[qn]
            self.eng[e].wait_ge(Q["sems"][i % Q["K"]], 16 * (i // Q["K"] + 1))
            self.seen_dma[e].add(tok)

    @staticmethod
    def _deps(r, w):
        toks = []
        for b in r:
            if b.lw is not None:
                toks.append(b.lw)
        for b in w:
            if b.lw is not None:
                toks.append(b.lw)
            toks.extend(b.rd.values())
        return toks

    @staticmethod
    def _commit(tok, key, r, w):
        for b in w:
            b.lw = tok
            b.rd = {}
        for b in r:
            if b not in w:
                b.rd[key] = tok

    def op(self, e, fn, r=(), w=()):
        for t in self._deps(r, w):
            self._wait(e, t)
        inst = fn()
        self.cnt[e] += 1
        c = self.cnt[e]
        sem, _ = self._sem_for(e, c)
        inst.then_inc(sem, 1)
        self._commit(("e", e, c), e, r, w)

    def dma(self, qn, out, in_, r=(), w=()):
        Q = self.q[qn]
        e = Q["issuer"]
        i = Q["n"]
        if i >= Q["K"]:
            self._wait(e, ("d", qn, i - Q["K"]))
        for t in self._deps(r, w):
            self._wait(e, t)
        inst = self.eng[e].dma_start(out=out, in_=in_)
        inst.then_inc(Q["sems"][i % Q["K"]], 16)
        Q["n"] += 1
        tok = ("d", qn, i)
        self._commit(tok, tok, r, w)

    def barrier(self):
        for e in self.eng:
            for x in self.eng:
                if x != e and self.cnt[x] > 0:
                    self._wait(e, ("e", x, self.cnt[x]))
            for qn, Q in self.q.items():
                for i in range(max(0, Q["n"] - Q["K"]), Q["n"]):
                    self._wait(e, ("d", qn, i))

    def rot(self, key, lst):
        i = self.rr.get(key, 0)
        self.rr[key] = i + 1
        return lst[i % len(lst)]


def _rel_bucket_np(dist):
    max_exact = 16
    n = np.maximum(dist, 0)
    nf = np.maximum(n, 1).astype(np.float32)
    large = max_exact + (np.log(nf / np.float32(max_exact)) / np.float32(math.log(2048 / max_exact))
                         * np.float32(32 - max_exact)).astype(np.int32)
    large = np.minimum(large, 31)
    return np.where(n < max_exact, n, large)


def _host_consts():
    bf = ml_dtypes.bfloat16
    idx = np.arange(128)
    ident = np.eye(128, dtype=np.float32)
    jrev = ident[::-1].copy()
    trineg = -(idx[:, None] >= idx[None, :]).astype(np.float32)
    maskb = (idx[:, None] < idx[None, :]).astype(np.float32)
    dist = np.arange(NDIST) - 127
    bucket = _rel_bucket_np(dist)
    oh = np.zeros((32, NDIST), np.float32)
    oh[bucket, np.arange(NDIST)] = 1.0
    mult = np.zeros((12, NDIST), np.float32)
    ge0 = dist >= 0
    ma = ((dist <= 128) & ge0).astype(np.float32) + ((dist % 4 == 0) & (dist <= 512) & ge0) + ((dist % 16 == 0) & ge0)
    mult[0:4] = ma
    mult[4:8] = (ge0 & (dist <= 127)).astype(np.float32)
    mult[8:12] = ge0.astype(np.float32)
    return {
        "c_ident": ident.astype(bf),
        "c_jrev": jrev,
        "c_trineg": trineg.astype(bf),
        "c_maskb": maskb.astype(bf),
        "c_oh": oh,
        "c_mult": mult,
    }


def build_program(n_seq, n_layers=DEPTH, dbg=False):
    nc = bass.Bass("TRN2", target_bir_lowering=False)
    es = contextlib.ExitStack()
    S = Sched(nc, es)
    S.add_queue("sp", "sp", K=8)
    S.add_queue("pool", "pool", K=4)

    def dram(name, shape, dt, kind):
        return nc.dram_tensor(name, list(shape), dt, kind=kind)

    x_in = dram("x", [n_seq, SEQ, D], F32, "ExternalInput")
    mem_in = dram("mem", [n_seq, MEM, D], F32, "ExternalInput")
    rel_in = dram("rel_table", [32, 12], F32, "ExternalInput")
    gvec = {k: dram(k, [DEPTH, D], F32, "ExternalInput") for k in ("g_mix", "g_group", "g_cross", "g_mem", "g_mlp")}
    gfin_in = dram("g_final", [1, D], F32, "ExternalInput")
    gT_in = {k: dram(k, [DEPTH, 128, 8], F32, "ExternalInput") for k in ("gT_cross", "gT_mlp")}
    sinks_in = dram("sinks", [1, DEPTH * 4], F32, "ExternalInput")
    w_in_d = dram("w_in", [DEPTH, D, IN_W], F32, "ExternalInput")
    w_out_d = dram("w_out", [DEPTH, D, D], F32, "ExternalInput")
    w_xq_d = dram("w_xq", [DEPTH, D, 256], F32, "ExternalInput")
    w_xkv_d = dram("w_xkv", [DEPTH, D, 512], F32, "ExternalInput")
    w_xo_d = dram("w_xo", [DEPTH, 256, D], F32, "ExternalInput")
    w_up_d = dram("w_up", [DEPTH, D, DFF], F32, "ExternalInput")
    w_dn_d = dram("w_down", [DEPTH, DFF, D], F32, "ExternalInput")
    c_ident = dram("c_ident", [128, 128], BF16, "ExternalInput")
    c_jrev = dram("c_jrev", [128, 128], F32, "ExternalInput")
    c_trineg = dram("c_trineg", [128, 128], BF16, "ExternalInput")
    c_maskb = dram("c_maskb", [128, 128], BF16, "ExternalInput")
    c_oh = dram("c_oh", [32, NDIST], F32, "ExternalInput")
    c_mult = dram("c_mult", [12, NDIST], F32, "ExternalInput")
    y_out = dram("y", [n_seq, SEQ, D], F32, "ExternalOutput")
    xA = dram("xA", [n_seq, SEQ, D], F32, "Internal")
    xB = dram("xB", [n_seq, SEQ, D], F32, "Internal")
    Fd = dram("Fd", [12, NDIST + 1], F32, "Internal")
    Texp = dram("Texp", [12, 128, SEQ], BF16, "Internal")
    dbg_out = {}
    if dbg:
        dbg_out["x1"] = dram("dbg_x1", [n_seq, SEQ, D], F32, "ExternalOutput")
        dbg_out["yg"] = dram("dbg_yg", [4, SEQ, 256], F32, "ExternalOutput")

    def dbuf(name):
        return [[Buf(f"{name}_{s}_{i}") for i in range(NT)] for s in range(n_seq)]

    xA_b, xB_b, y_b = dbuf("xA"), dbuf("xB"), dbuf("y")
    Texp_b = [Buf(f"Texp{h}") for h in range(12)]
    none_b = []

    uid = [0]

    def sb(stack, name, shape, dt):
        uid[0] += 1
        name = f"{name}_u{uid[0]}"
        t = stack.enter_context(nc.sbuf_tensor(name, list(shape), dt))
        return Tile(t, Buf(name))

    def sbn(stack, name, shape, dt, n):
        return [sb(stack, f"{name}{i}", shape, dt) for i in range(n)]

    PS = []
    for i in range(8):
        t = es.enter_context(nc.psum_tensor(f"ps{i}", [128, 512], F32))
        PS.append(Tile(t, Buf(f"ps{i}")))
    PS_S = [PS[0], PS[1]]
    PS_O = [PS[2], PS[3]]
    PS_M = [PS[4], PS[5]]
    PS_T = PS[6]
    PS_X = PS[7]

    ident = sb(es, "ident", [128, 128], BF16)
    trineg = sb(es, "trineg", [128, 128], BF16)
    maskb = sb(es, "maskb", [128, 128], BF16)
    onescol = sb(es, "onescol", [128, 2], BF16)
    expsink = sb(es, "expsink", [128, DEPTH * 4], F32)
    epsc = sb(es, "epsc", [128, 1], F32)

    act = nc.scalar
    dve = nc.vector
    pool = nc.gpsimd
    pe = nc.tensor

    with contextlib.ExitStack() as ps_:
        S.dma("sp", ident.t[:], c_ident.ap(), w=[ident.b])
        S.dma("sp", trineg.t[:], c_trineg.ap(), w=[trineg.b])
        S.dma("sp", maskb.t[:], c_maskb.ap(), w=[maskb.b])
        S.op("dve", lambda: dve.memset(onescol.t[:], 1.0), w=[onescol.b])
        S.op("dve", lambda: dve.memset(epsc.t[:], EPS), w=[epsc.b])
        S.dma("sp", expsink.t[:], sinks_in.ap()[0:1, :].partition_broadcast(128), w=[expsink.b])
        S.op("act", lambda: act.activation(out=expsink.t[:], in_=expsink.t[:], func=AF.Exp), r=[expsink.b], w=[expsink.b])

        tab = sb(ps_, "tab", [32, 12], F32)
        oh = sb(ps_, "oh", [32, NDIST], F32)
        mu = sb(ps_, "mu", [12, NDIST], F32)
        Fs = sb(ps_, "Fs", [12, NDIST + 1], F32)
        jrev = sb(ps_, "jrev", [128, 128], F32)
        Xs = sbn(ps_, "Xs", [128, 512], F32, 2)
        Tb = sbn(ps_, "Tb", [128, 512], BF16, 2)
        S.dma("sp", tab.t[:], rel_in.ap(), w=[tab.b])
        S.dma("sp", oh.t[:], c_oh.ap(), w=[oh.b])
        S.dma("sp", mu.t[:], c_mult.ap(), w=[mu.b])
        S.dma("sp", jrev.t[:], c_jrev.ap(), w=[jrev.b])
        S.op("dve", lambda: dve.memset(Fs.t[:], 0.0), w=[Fs.b])
        for ch in range((NDIST + 511) // 512):
            c0 = ch * 512
            n = min(512, NDIST - c0)
            pb = S.rot("psm", PS_M)
            S.op("pe", lambda: pe.matmul(pb.t[0:12, 0:n], lhsT=tab.t[:, :], rhs=oh.t[:, c0:c0 + n], start=True, stop=True),
                 r=[tab.b, oh.b], w=[pb.b])
            S.op("act", lambda: act.activation(out=Fs.t[:, c0:c0 + n], in_=pb.t[0:12, 0:n], func=AF.Exp), w=[pb.b, Fs.b])
            S.op("dve", lambda: dve.tensor_tensor(out=Fs.t[:, c0:c0 + n], in0=Fs.t[:, c0:c0 + n], in1=mu.t[:, c0:c0 + n], op=ALU.mult),
                 r=[mu.b], w=[Fs.b])
        Fd_b = Buf("Fd")
        S.dma("sp", Fd.ap(), Fs.t[:], r=[Fs.b], w=[Fd_b])
        for rh in range(12):
            W = 256 if 4 <= rh < 8 else SEQ
            for c0 in range(0, W, 512):
                n = min(512, W - c0)
                X = S.rot("Xs", Xs)
                T_ = S.rot("Tb", Tb)
                src = bass.AP(Fd, rh * (NDIST + 1) + c0, [[1, 128], [1, n]])
                S.dma("sp", X.t[:, 0:n], src, r=[Fd_b], w=[X.b])
                pb = S.rot("psm", PS_M)
                S.op("pe", lambda: pe.matmul(pb.t[:, 0:n], lhsT=jrev.t[:], rhs=X.t[:, 0:n], start=True, stop=True),
                     r=[jrev.b, X.b], w=[pb.b])
                S.op("dve", lambda: dve.tensor_copy(out=T_.t[:, 0:n], in_=pb.t[:, 0:n]), w=[pb.b, T_.b])
                S.dma("sp", Texp.ap()[rh, :, c0:c0 + n], T_.t[:, 0:n], r=[T_.b], w=[Texp_b[rh]])
        S.barrier()

    def load_gain(tile_, src_handle, row):
        S.dma("sp", tile_.t[:], src_handle.ap()[row:row + 1, :].partition_broadcast(128), w=[tile_.b])

    def w_view(handle, l, rows0, nrows):
        return handle.ap()[l, rows0:rows0 + nrows, :].rearrange("(c p) n -> p c n", p=128)

    def rms_rstd(x_ap, ncols, junk, ss_t, ss_b, rstd_t, rstd_b, xb):
        S.op("act", lambda: act.activation(out=junk.t[:, 0:ncols], in_=x_ap, func=AF.Square, accum_out=ss_t),
             r=[xb], w=[junk.b, ss_b])
        S.op("act", lambda: act.activation(out=rstd_t, in_=ss_t, func=AF.Ln, scale=1.0 / ncols, bias=epsc.t[:, 0:1]),
             r=[ss_b, epsc.b], w=[rstd_b])
        S.op("act", lambda: act.activation(out=rstd_t, in_=rstd_t, func=AF.Exp, scale=-0.5), r=[rstd_b], w=[rstd_b])

    def transpose_to(dst_ap3, dst_b, src_tile, nchunks, eng="dve"):
        pb = PS_T.t.bitcast(BF16)
        for c in range(nchunks):
            S.op("pe", lambda: pe.transpose(out=pb[:, c * 128:(c + 1) * 128], in_=src_tile.t[:, c * 128:(c + 1) * 128],
                                            identity=ident.t[:]),
                 r=[src_tile.b, ident.b], w=[PS_T.b])
        srcv = pb[:, 0:nchunks * 128].rearrange("p (k t) -> p k t", t=128)
        if eng == "dve":
            S.op("dve", lambda: dve.tensor_copy(out=dst_ap3, in_=srcv), w=[PS_T.b, dst_b])
        else:
            S.op("act", lambda: act.copy(out=dst_ap3, in_=srcv), w=[PS_T.b, dst_b])

    kxT = sb(es, "kxT", [128, n_seq, 2, MEM], BF16)
    vxa = sb(es, "vxa", [128, n_seq, 2, 4, 65], BF16)
    for l in range(n_layers):
        x_src, x_src_b = (x_in, None) if l == 0 else (xB, xB_b)
        last = l == n_layers - 1

        with contextlib.ExitStack() as ms:
            gMix = sb(ms, "gMix", [128, D], F32)
            gGrp = sb(ms, "gGrp", [128, D], F32)
            hT = sb(ms, "hT", [128, 8, SEQ], BF16)
            wsl = sb(ms, "wsl", [128, 8, 768], BF16)
            wout = sb(ms, "wout", [128, 8, D], BF16)
            Ttab = sb(ms, "Ttab", [128, 4, SEQ], BF16)
            TtabC = sb(ms, "TtabC", [128, 4, 256], BF16)
            qT = sb(ms, "qT", [128, 4, SEQ], BF16)
            kT = sb(ms, "kT", [128, 2, SEQ], BF16)
            vaug = sb(ms, "vaug", [128, NT, 4, 65], BF16)
            yg = sb(ms, "yg", [128, NT, 256], F32)
            yT = sb(ms, "yT", [128, 8, SEQ], BF16)
            xts = sbn(ms, "xt", [128, D], F32, 2)
            xns = sbn(ms, "xn", [128, D], BF16, 2)
            junk = sb(ms, "junk", [128, D], BF16)
            Ebufs = sbn(ms, "Eb", [128, 512], BF16, 3)
            Pbufs = sbn(ms, "Pb", [128, 512], BF16, 4)
            Ubufs = sbn(ms, "Ub", [128, 512], F32, 2)
            Wbufs = sbn(ms, "Wb", [128, 512], BF16, 3)
            tmpc = sb(ms, "tmpc", [128, 4, 65], F32)
            Osb = sb(ms, "Osb", [128, 4, 65], F32)
            ssx = sb(ms, "ssx", [128, 2], F32)
            rsx = sb(ms, "rsx", [128, 2], F32)
            ssg = sb(ms, "ssg", [128, NT], F32)
            rsg = sb(ms, "rsg", [128, NT], F32)
            dden = sb(ms, "dden", [128, 4], F32)
            Rb = sb(ms, "Rb", [128, 4], F32)
            Cbs = sbn(ms, "Cb", [128, 4], F32, 2)
            gate = sb(ms, "gate", [128, 16], F32)
            top8 = sb(ms, "top8", [128, 8], F32)
            Msel = sb(ms, "Msel", [128, 4, NT, 8], F32)
            ksum = sb(ms, "ksum", [128, 2, 8], F32)
            ksumb = sb(ms, "ksumb", [128, 2, 8], BF16)

            load_gain(gMix, gvec["g_mix"], l)
            load_gain(gGrp, gvec["g_group"], l)
            S.dma("pool", wout.t[:], w_view(w_out_d, l, 0, D), w=[wout.b])
            S.dma("sp", TtabC.t[:], Texp.ap()[4:8, :, 0:256].rearrange("h p t -> p h t"), r=Texp_b[4:8], w=[TtabC.b])
            S.op("dve", lambda: dve.memset(vaug.t[:], 1.0), w=[vaug.b])
            S.op("pool", lambda: pool.memset(qT.t[:], 0.0), w=[qT.b])

            def load_wslice(col0, ncols):
                S.dma("pool", wsl.t[:, :, 0:ncols],
                      w_in_d.ap()[l, :, col0:col0 + ncols].rearrange("(c p) n -> p c n", p=128), w=[wsl.b])

            def proj_T(dst, nchunk, wcol0, scale, ei):
                return [(lambda cc=cc, tg=tg: proj_T_chunk(dst, cc, tg, wcol0, scale, ei)) for cc in range(nchunk) for tg in range(4)]

            def proj_T_chunk(dst, cc, tg, wcol0, scale, ei):
                if True:
                    if True:
                        pb = S.rot("psm", PS_M)
                        for c in range(8):
                            S.op("pe", lambda: pe.matmul(pb.t[:, :], lhsT=wsl.t[:, c, wcol0 + cc * 128: wcol0 + (cc + 1) * 128],
                                                         rhs=hT.t[:, c, tg * 512:(tg + 1) * 512], start=(c == 0), stop=(c == 7)),
                                 r=[wsl.b, hT.b], w=[pb.b])
                        ei[0] += 1
                        if dst is qT:
                            S.op("dve", lambda: dve.tensor_scalar(out=qT.t[0:64, 2 * cc, tg * 512:(tg + 1) * 512], in0=pb.t[0:64, :],
                                                                  scalar1=scale, scalar2=None, op0=ALU.mult),
                                 w=[pb.b, dst.b])
                            S.op("act", lambda: act.activation(out=qT.t[64:128, 2 * cc + 1, tg * 512:(tg + 1) * 512], in_=pb.t[64:128, :],
                                                               func=AF.Copy, scale=scale), w=[pb.b, dst.b])
                        elif ei[0] % 2 == 0:
                            S.op("dve", lambda: dve.tensor_scalar(out=dst.t[:, cc, tg * 512:(tg + 1) * 512], in0=pb.t[:, :],
                                                                  scalar1=scale, scalar2=None, op0=ALU.mult),
                                 w=[pb.b, dst.b])
                        else:
                            S.op("act", lambda: act.activation(out=dst.t[:, cc, tg * 512:(tg + 1) * 512], in_=pb.t[:, :],
                                                               func=AF.Copy, scale=scale), w=[pb.b, dst.b])

            def proj_V(wcol0, nh, ei):
                return [(lambda i=i: proj_V_chunk(wcol0, nh, ei, i)) for i in range(NT)]

            def proj_V_chunk(wcol0, nh, ei, i):
                if True:
                    pb = S.rot("psm", PS_M)
                    for c in range(8):
                        S.op("pe", lambda: pe.matmul(pb.t[:, 0:nh * 64], lhsT=hT.t[:, c, i * 128:(i + 1) * 128],
                                                     rhs=wsl.t[:, c, wcol0:wcol0 + nh * 64], start=(c == 0), stop=(c == 7)),
                             r=[wsl.b, hT.b], w=[pb.b])
                    srcv = pb.t[:, 0:nh * 64].rearrange("p (h e) -> p h e", e=64)
                    ei[0] += 1
                    if ei[0] % 2 == 0:
                        S.op("dve", lambda: dve.tensor_copy(out=vaug.t[:, i, 0:nh, 0:64], in_=srcv), w=[pb.b, vaug.b])
                    else:
                        S.op("act", lambda: act.copy(out=vaug.t[:, i, 0:nh, 0:64], in_=srcv), w=[pb.b, vaug.b])

            mulrr = [0]

            def mul_T(out_ap, in0_ap, in1_ap, r, w):
                mulrr[0] += 1
                if mulrr[0] % 2 == 0:
                    S.op("dve", lambda: dve.tensor_tensor(out=out_ap, in0=in0_ap, in1=in1_ap, op=ALU.mult), r=r, w=w)
                else:
                    S.op("pool", lambda: pool.tensor_tensor(out=out_ap, in0=in0_ap, in1=in1_ap, op=ALU.mult), r=r, w=w)

            def finish_group(g, Ob, h, extra=None, add_osb=False):
                Ov = Ob.t[:, 0:260].rearrange("p (j e) -> p j e", e=65)
                if add_osb:
                    S.op("dve", lambda: dve.tensor_tensor(out=Osb.t[:], in0=Ov, in1=Osb.t[:], op=ALU.add), w=[Ob.b, Osb.b])
                    src, srcb = Osb.t, Osb.b
                    den_ap = Osb.t[:, :, 64]
                else:
                    src, srcb = None, Ob.b
                    den_ap = Ov[:, :, 64]
                if extra is not None:
                    S.op("dve", lambda: dve.tensor_scalar(out=dden.t[:], in0=den_ap, scalar1=extra, scalar2=None, op0=ALU.add),
                         r=[expsink.b], w=[srcb, dden.b])
                    S.op("dve", lambda: dve.reciprocal(out=dden.t[:], in_=dden.t[:]), w=[dden.b])
                else:
                    S.op("dve", lambda: dve.reciprocal(out=dden.t[:], in_=den_ap), w=[srcb, dden.b])
                sap = Osb.t[:, :, 0:64] if add_osb else Ov[:, :, 0:64]
                dbc = bass.AP(dden.t, 0, [[4, 128], [1, 4], [0, 64]])
                S.op("dve", lambda: dve.tensor_tensor(out=yg.t[:, 4 * g:4 * g + 4, h * 64:(h + 1) * 64], in0=sap, in1=dbc, op=ALU.mult),
                     r=[dden.b], w=[srcb, yg.b])

            SB3 = [PS[0], PS[1], PS[4]]
            OB2 = [PS[2], PS[3]]
            PVB = [PS[5], PS[7]]

            def run_pipeline(steps, lags):
                n = len(steps)
                offs = [0]
                for lg in lags:
                    offs.append(offs[-1] + lg)
                for t in range(n + offs[-1]):
                    for si, o in enumerate(offs):
                        k = t - o
                        if 0 <= k < n:
                            steps[k][si]()

            def softmax_steps(h, kp, kc, qp, qc, vh, Tap, Tb_, window, extra=None):
                steps = []
                for g in range(4):
                    grp = {"Ob": None, "first": True}
                    bl = []
                    for b in range(max(0, 4 * g - window), 4 * g + 4):
                        jlo = max(0, b - 4 * g)
                        jhi = min(3, b + window - 4 * g)
                        if jlo <= jhi:
                            bl.append((b, jlo, jhi))
                    for idx, (b, jlo, jhi) in enumerate(bl):
                        st = {}
                        last = idx == len(bl) - 1

                        def F(st=st, b=b, jlo=jlo, jhi=jhi, g=g):
                            ncol = (jhi - jlo + 1) * 128
                            q0 = (4 * g + jlo) * 128
                            Sb_ = S.rot("sb3", SB3)
                            S.op("pe", lambda: pe.matmul(Sb_.t[:, 0:ncol], lhsT=kT.t[:, kc, b * 128:(b + 1) * 128],
                                                         rhs=qT.t[:, qc, q0:q0 + ncol], start=True, stop=True),
                                 r=[kT.b, qT.b], w=[Sb_.b])
                            e_ = S.rot("eb", Ebufs)
                            S.op("act", lambda: act.activation(out=e_.t[:, 0:ncol], in_=Sb_.t[:, 0:ncol], func=AF.Exp), w=[Sb_.b, e_.b])
                            p_ = S.rot("pb", Pbufs)
                            tau0 = q0 - 128 * b
                            S.op("dve", lambda: dve.tensor_tensor(out=p_.t[:, 0:ncol], in0=e_.t[:, 0:ncol], in1=Tap[:, tau0:tau0 + ncol], op=ALU.mult),
                                 r=[e_.b, Tb_], w=[p_.b])
                            st["p"] = p_

                        def B(st=st, b=b, jlo=jlo, jhi=jhi, g=g, grp=grp, last=last):
                            if grp["Ob"] is None:
                                grp["Ob"] = S.rot("ob2", OB2)
                            Ob = grp["Ob"]
                            p_ = st["p"]
                            for j in range(jlo, jhi + 1):
                                stf = grp["first"]
                                S.op("pe", lambda: pe.matmul(Ob.t[:, j * 65:(j + 1) * 65], lhsT=p_.t[:, (j - jlo) * 128:(j - jlo + 1) * 128],
                                                             rhs=vaug.t[:, b, vh, :], start=stf, stop=False, skip_group_check=True),
                                     r=[p_.b, vaug.b], w=[Ob.b])
                                grp["first"] = False
                            if last:
                                finish_group(g, Ob, h, extra=extra)

                        steps.append((F, B))
                return steps

            def stick_steps(h, kp, kc):
                steps = []
                for g in range(4):
                    for b in range(4 * g + 3, -1, -1):
                        st = {}
                        firstg = b == 4 * g + 3
                        lastg = b == 0

                        def S1(st=st, b=b, g=g):
                            jlo = max(0, b - 4 * g)
                            ncol = (4 - jlo) * 128
                            q0 = (4 * g + jlo) * 128
                            diag = b >= 4 * g
                            Zb = S.rot("sb3", SB3)
                            S.op("pe", lambda: pe.matmul(Zb.t[:, 0:ncol], lhsT=kT.t[:, kc, b * 128:(b + 1) * 128],
                                                         rhs=qT.t[:, h, q0:q0 + ncol], start=True, stop=False,
                                                         skip_group_check=True),
                                 r=[kT.b, qT.b], w=[Zb.b])
                            u_ = S.rot("ub", Ubufs)
                            S.op("act", lambda: act.activation(out=u_.t[:, 0:ncol], in_=Zb.t[:, 0:ncol], func=AF.Exp), w=[Zb.b, u_.b])
                            w_ = S.rot("wb", Wbufs)
                            S.op("act", lambda: act.activation(out=w_.t[:, 0:ncol], in_=u_.t[:, 0:ncol], func=AF.Ln, bias=1.0),
                                 r=[u_.b], w=[w_.b])
                            if diag:
                                S.op("pool", lambda: pool.tensor_tensor(out=w_.t[:, 0:128], in0=w_.t[:, 0:128], in1=maskb.t[:], op=ALU.mult),
                                     r=[maskb.b], w=[w_.b])
                            st.update(Zb=Zb, w=w_, jlo=jlo, ncol=ncol, diag=diag)

                        def S2(st=st):
                            Zb, w_, ncol = st["Zb"], st["w"], st["ncol"]
                            S.op("pe", lambda: pe.matmul(Zb.t[:, 0:ncol], lhsT=trineg.t[:], rhs=w_.t[:, 0:ncol], start=False, stop=True,
                                                         skip_group_check=True),
                                 r=[trineg.b, w_.b], w=[Zb.b])
                            p_ = S.rot("pb", Pbufs)
                            S.op("act", lambda: act.activation(out=p_.t[:, 0:ncol], in_=Zb.t[:, 0:ncol], func=AF.Exp), w=[Zb.b, p_.b])
                            if st["diag"]:
                                S.op("pool", lambda: pool.tensor_tensor(out=p_.t[:, 0:128], in0=p_.t[:, 0:128], in1=maskb.t[:], op=ALU.mult),
                                     r=[maskb.b], w=[p_.b])
                            st["p"] = p_

                        def S3(st=st, b=b, g=g, firstg=firstg, lastg=lastg):
                            p_, w_, jlo = st["p"], st["w"], st["jlo"]
                            if firstg:
                                S.op("dve", lambda: dve.memset(Osb.t[:], 0.0), w=[Osb.b])
                                S.op("dve", lambda: dve.memset(Rb.t[:], 0.0), w=[Rb.b])
                                S.op("dve", lambda: dve.memset(Cbs[0].t[:], 1.0), w=[Cbs[0].b])
                                S.op("dve", lambda: dve.memset(Cbs[1].t[:], 1.0), w=[Cbs[1].b])
                            kk = (4 * g + 3 - b) % 2
                            Cc, Cn = Cbs[kk], Cbs[1 - kk]
                            Pv = S.rot("pvb", PVB)
                            first = True
                            for j in range(jlo, 4):
                                stf = first
                                S.op("pe", lambda: pe.matmul(Pv.t[:, j * 64:(j + 1) * 64], lhsT=p_.t[:, (j - jlo) * 128:(j - jlo + 1) * 128],
                                                             rhs=vaug.t[:, b, h, 0:64], start=stf, stop=False, skip_group_check=True),
                                     r=[p_.b, vaug.b], w=[Pv.b])
                                first = False
                                if not lastg:
                                    S.op("pe", lambda: pe.matmul(Pv.t[:, 256 + j:257 + j], lhsT=w_.t[:, (j - jlo) * 128:(j - jlo + 1) * 128],
                                                                 rhs=onescol.t[:, 0:1], start=False, stop=False, skip_group_check=True),
                                         r=[w_.b, onescol.b], w=[Pv.b])
                            if not lastg:
                                S.op("dve", lambda: dve.tensor_tensor(out=Rb.t[:, jlo:4], in0=Pv.t[:, 256 + jlo:260], in1=Rb.t[:, jlo:4], op=ALU.add),
                                     w=[Pv.b, Rb.b])
                                S.op("act", lambda: act.activation(out=Cn.t[:, jlo:4], in_=Rb.t[:, jlo:4], func=AF.Exp, scale=-1.0),
                                     r=[Rb.b], w=[Cn.b])
                            nj = 4 - jlo
                            cbc = bass.AP(Cc.t, jlo, [[4, 128], [1, nj], [0, 64]])
                            Pv3 = Pv.t[:, jlo * 64:256].rearrange("p (j e) -> p j e", e=64)
                            S.op("dve", lambda: dve.tensor_tensor(out=tmpc.t[:, jlo:4, 0:64], in0=Pv3, in1=cbc, op=ALU.mult),
                                 r=[Cc.b], w=[Pv.b, tmpc.b])
                            S.op("dve", lambda: dve.tensor_tensor(out=Osb.t[:, jlo:4, 0:64], in0=tmpc.t[:, jlo:4, 0:64], in1=Osb.t[:, jlo:4, 0:64],
                                                                  op=ALU.add),
                                 r=[tmpc.b], w=[Osb.b])
                            if lastg:
                                S.op("pool", lambda: pool.tensor_copy(out=yg.t[:, 4 * g:4 * g + 4, h * 64:(h + 1) * 64], in_=Osb.t[:, :, 0:64]),
                                     r=[Osb.b], w=[yg.b])

                        steps.append((S1, S2, S3))
                return steps

            def moba_gates(h, kp, kc):
                for i in range(8, NT):
                    qb = i // 2
                    S.op("pe", lambda: pe.matmul(PS[6].t[:, 0:8], lhsT=qT.t[:, h, i * 128:(i + 1) * 128],
                                                 rhs=ksumb.t[:, kc, :], start=True, stop=True),
                         r=[qT.b, ksumb.b], w=[PS[6].b])
                    S.op("dve", lambda: dve.memset(gate.t[:], -BIG), w=[gate.b])
                    S.op("dve", lambda: dve.tensor_copy(out=gate.t[:, 0:qb], in_=PS[6].t[:, 0:qb]), w=[PS[6].b, gate.b])
                    S.op("dve", lambda: dve.max(out=top8.t[:], in_=gate.t[:]), r=[gate.b], w=[top8.b])
                    S.op("dve", lambda: dve.tensor_scalar(out=Msel.t[:, h, i, :], in0=gate.t[:, 0:8], scalar1=top8.t[:, 2:3], scalar2=None,
                                                          op0=ALU.is_ge),
                         r=[gate.b, top8.b], w=[Msel.b])

            def moba_steps(h, kp, kc):
                Tap = Ttab.t[:, h, :]
                steps = []
                for g in range(4):
                    grp = {"Own": None, "first_own": True, "Pvp": None, "first_pv": True}
                    for b in range(0, 4 * g + 4):
                        st = {}
                        firstg = b == 0
                        lastg = b == 4 * g + 3

                        def F(st=st, b=b, g=g):
                            jlo = max(0, b - 4 * g)
                            ncol = (4 - jlo) * 128
                            q0 = (4 * g + jlo) * 128
                            Sb_ = S.rot("sb3", SB3)
                            S.op("pe", lambda: pe.matmul(Sb_.t[:, 0:ncol], lhsT=kT.t[:, kc, b * 128:(b + 1) * 128],
                                                         rhs=qT.t[:, h, q0:q0 + ncol], start=True, stop=True),
                                 r=[kT.b, qT.b], w=[Sb_.b])
                            e_ = S.rot("eb", Ebufs)
                            S.op("act", lambda: act.activation(out=e_.t[:, 0:ncol], in_=Sb_.t[:, 0:ncol], func=AF.Exp), w=[Sb_.b, e_.b])
                            p_ = S.rot("pb", Pbufs)
                            tau0 = q0 - 128 * b
                            S.op("dve", lambda: dve.tensor_tensor(out=p_.t[:, 0:ncol], in0=e_.t[:, 0:ncol], in1=Tap[:, tau0:tau0 + ncol], op=ALU.mult),
                                 r=[e_.b, Ttab.b], w=[p_.b])
                            st.update(p=p_, jlo=jlo)

                        def B(st=st, b=b, g=g, grp=grp, firstg=firstg, lastg=lastg):
                            p_, jlo = st["p"], st["jlo"]
                            n = b // 2
                            if firstg:
                                grp["Own"] = S.rot("ob2", OB2)
                                S.op("dve", lambda: dve.memset(Osb.t[:], 0.0), w=[Osb.b])
                            if b % 2 == 0:
                                grp["Pvp"] = S.rot("pvb", PVB)
                                grp["first_pv"] = True
                            Own, Pvp = grp["Own"], grp["Pvp"]
                            for j in range(jlo, 4):
                                qb = (4 * g + j) // 2
                                if n == qb or g < 2:
                                    stf = grp["first_own"]
                                    S.op("pe", lambda: pe.matmul(Own.t[:, j * 65:(j + 1) * 65], lhsT=p_.t[:, (j - jlo) * 128:(j - jlo + 1) * 128],
                                                                 rhs=vaug.t[:, b, h, :], start=stf, stop=False, skip_group_check=True),
                                         r=[p_.b, vaug.b], w=[Own.b])
                                    grp["first_own"] = False
                                else:
                                    stf = grp["first_pv"]
                                    S.op("pe", lambda: pe.matmul(Pvp.t[:, j * 65:(j + 1) * 65], lhsT=p_.t[:, (j - jlo) * 128:(j - jlo + 1) * 128],
                                                                 rhs=vaug.t[:, b, h, :], start=stf, stop=False, skip_group_check=True),
                                         r=[p_.b, vaug.b], w=[Pvp.b])
                                    grp["first_pv"] = False
                            if b % 2 == 1 and g >= 2:
                                jA = 0 if n < 2 * g else 2
                                if n < 2 * g + 1:
                                    nj = 4 - jA
                                    Mbc = bass.AP(Msel.t, ((h * NT + 4 * g + jA) * 8 + n), [[4 * NT * 8, 128], [8, nj], [0, 65]])
                                    Pv3 = Pvp.t[:, jA * 65:4 * 65].rearrange("p (j e) -> p j e", e=65)
                                    S.op("dve", lambda: dve.tensor_tensor(out=tmpc.t[:, jA:4, :], in0=Pv3, in1=Mbc, op=ALU.mult),
                                         r=[Msel.b], w=[Pvp.b, tmpc.b])
                                    S.op("dve", lambda: dve.tensor_tensor(out=Osb.t[:, jA:4, :], in0=tmpc.t[:, jA:4, :], in1=Osb.t[:, jA:4, :],
                                                                          op=ALU.add),
                                         r=[tmpc.b], w=[Osb.b])
                            if lastg:
                                finish_group(g, Own, h, add_osb=True)

                        steps.append((F, B))
                return steps

            def group_norm(m):
                pieces = []

                def sq(i):
                    def f():
                        S.op("act", lambda: act.activation(out=junk.t[:, 0:256], in_=yg.t[:, i, :], func=AF.Square, accum_out=ssg.t[:, i:i + 1]),
                             r=[yg.b], w=[junk.b, ssg.b])
                    return f

                def stats():
                    S.op("act", lambda: act.activation(out=rsg.t[:], in_=ssg.t[:], func=AF.Ln, scale=1.0 / 256, bias=epsc.t[:, 0:1]),
                         r=[ssg.b, epsc.b], w=[rsg.b])
                    S.op("act", lambda: act.activation(out=rsg.t[:], in_=rsg.t[:], func=AF.Exp, scale=-0.5), w=[rsg.b])

                def tr(i):
                    def f():
                        xn = S.rot("xn", xns)
                        S.op("dve", lambda: dve.scalar_tensor_tensor(out=xn.t[:, 0:256], in0=yg.t[:, i, :], scalar=rsg.t[:, i:i + 1],
                                                                     in1=gGrp.t[:, m * 256:(m + 1) * 256], op0=ALU.mult, op1=ALU.mult),
                             r=[yg.b, rsg.b, gGrp.b], w=[xn.b])
                        transpose_to(yT.t[:, 2 * m:2 * m + 2, i * 128:(i + 1) * 128], yT.b, xn, 2, eng=("dve" if i % 2 else "act"))
                    return f

                for i in range(0, NT, 4):
                    pieces.append(lambda i=i: [sq(j)() for j in range(i, i + 4)])
                pieces.append(stats)
                for i in range(NT):
                    pieces.append(tr(i))
                return pieces

            def interleave(main, side, every=2):
                si = 0
                for ci, ch in enumerate(main):
                    ch()
                    if ci % every == every - 1 and si < len(side):
                        side[si]()
                        si += 1
                while si < len(side):
                    side[si]()
                    si += 1

            for s in range(0 if _os.environ.get("T_SKIPMIX") else n_seq):
                for i in range(NT):
                    xt = S.rot("xt", xts)
                    S.dma("sp", xt.t[:], x_src.ap()[s, i * 128:(i + 1) * 128, :],
                          r=([x_src_b[s][i]] if x_src_b else none_b), w=[xt.b])
                    k2 = i % 2
                    rms_rstd(xt.t[:], D, junk, ssx.t[:, k2:k2 + 1], ssx.b, rsx.t[:, k2:k2 + 1], rsx.b, xt.b)
                    xn = S.rot("xn", xns)
                    S.op("dve", lambda: dve.scalar_tensor_tensor(out=xn.t[:], in0=xt.t[:], scalar=rsx.t[:, k2:k2 + 1], in1=gMix.t[:],
                                                                 op0=ALU.mult, op1=ALU.mult),
                         r=[xt.b, rsx.b, gMix.b], w=[xn.b])
                    transpose_to(hT.t[:, :, i * 128:(i + 1) * 128], hT.b, xn, 8, eng=("dve" if i % 2 else "act"))

                ei = [0]
                load_wslice(A_Q, 768)
                S.dma("sp", Ttab.t[:], Texp.ap()[0:4].rearrange("h p t -> p h t"), r=Texp_b[0:4], w=[Ttab.b])
                interleave(proj_T(qT, 2, 0, 0.125, ei) + proj_T(kT, 2, 256, 1.0, ei) + proj_V(512, 4, ei), [])
                load_wslice(B_Q, 768)
                steps = []
                for h in range(4):
                    steps += softmax_steps(h, (h % 2) * 64, h // 2, (h % 2) * 64, h, h, Ttab.t[:, h, :], Ttab.b, 15)
                run_pipeline(steps, [3])
                S.dma("sp", Ttab.t[:], Texp.ap()[8:12].rearrange("h p t -> p h t"), r=Texp_b[8:12], w=[Ttab.b])
                gn_pending = group_norm(0)
                if dbg and s == 0 and l == 0:
                    S.dma("sp", dbg_out["yg"].ap()[0].rearrange("(i p) f -> p i f", p=128), yg.t[:], r=[yg.b])
                interleave(proj_T(qT, 2, 0, 0.125, ei) + proj_T(kT, 2, 256, 1.0, ei) + proj_V(512, 4, ei), gn_pending)
                load_wslice(C_Q, 512)
                steps = []
                for h in range(4):
                    steps += stick_steps(h, (h % 2) * 64, h // 2)
                run_pipeline(steps, [1, 1])
                gn_pending = group_norm(1)
                if dbg and s == 0 and l == 0:
                    S.dma("sp", dbg_out["yg"].ap()[1].rearrange("(i p) f -> p i f", p=128), yg.t[:], r=[yg.b])
                interleave(proj_T(qT, 2, 0, 0.125, ei) + proj_T(kT, 1, 256, 1.0, ei) + proj_V(384, 2, ei), gn_pending)
                load_wslice(D_Q, 768)
                steps = []
                for h in range(4):
                    kvh = h // 2
                    qc = {0: 0, 2: 1, 1: 2, 3: 3}[h]
                    steps += softmax_steps(h, kvh * 64, 0, kvh * 64, qc, kvh, TtabC.t[:, h, :], TtabC.b, 1,
                                           extra=expsink.t[:, l * 4 + h:l * 4 + h + 1])
                run_pipeline(steps, [3])
                gn_pending = group_norm(2)
                if dbg and s == 0 and l == 0:
                    S.dma("sp", dbg_out["yg"].ap()[2].rearrange("(i p) f -> p i f", p=128), yg.t[:], r=[yg.b])
                interleave(proj_T(qT, 2, 0, 0.125, ei) + proj_T(kT, 2, 256, 1.0, ei) + proj_V(512, 4, ei), gn_pending)
                S.op("dve", lambda: dve.tensor_reduce(out=ksum.t[:], in_=kT.t[:].rearrange("p c (n k) -> p c n k", k=256),
                                                      axis=mybir.AxisListType.X, op=ALU.add),
                     r=[kT.b], w=[ksum.b])
                S.op("dve", lambda: dve.tensor_copy(out=ksumb.t[:], in_=ksum.t[:]), r=[ksum.b], w=[ksumb.b])
                steps = []
                for h in range(4):
                    moba_gates(h, (h % 2) * 64, h // 2)
                for h in range(4):
                    steps += moba_steps(h, (h % 2) * 64, h // 2)
                run_pipeline(steps, [3])
                interleave([], group_norm(3))
                if dbg and s == 0 and l == 0:
                    S.dma("sp", dbg_out["yg"].ap()[3].rearrange("(i p) f -> p i f", p=128), yg.t[:], r=[yg.b])
                OPB = [PS[4], PS[5], PS[0], PS[1]]
                xt_of = {}

                def load_x(i):
                    xt_ = S.rot("xt", xts)
                    S.dma("sp", xt_.t[:], x_src.ap()[s, i * 128:(i + 1) * 128, :],
                          r=([x_src_b[s][i]] if x_src_b else none_b), w=[xt_.b])
                    xt_of[i] = xt_

                load_x(0)
                for i in range(NT):
                    if i + 1 < NT:
                        load_x(i + 1)
                    xt = xt_of[i]
                    for hf in range(2):
                        pb = S.rot("opb", OPB)
                        for c in range(8):
                            S.op("pe", lambda: pe.matmul(pb.t[:, :], lhsT=yT.t[:, c, i * 128:(i + 1) * 128],
                                                         rhs=wout.t[:, c, hf * 512:(hf + 1) * 512], start=(c == 0), stop=(c == 7)),
                                 r=[yT.b, wout.b], w=[pb.b])
                        S.op("dve", lambda: dve.tensor_tensor(out=xt.t[:, hf * 512:(hf + 1) * 512], in0=pb.t[:, :],
                                                              in1=xt.t[:, hf * 512:(hf + 1) * 512], op=ALU.add),
                             w=[pb.b, xt.b])
                    S.dma("sp", xA.ap()[s, i * 128:(i + 1) * 128, :], xt.t[:], r=[xt.b], w=[xA_b[s][i]])
                    if dbg and l == 0:
                        S.dma("sp", dbg_out["x1"].ap()[s, i * 128:(i + 1) * 128, :], xt.t[:], r=[xt.b])
            S.barrier()

        with contextlib.ExitStack() as ks:
            gMem = sb(ks, "gMem", [128, D], F32)
            wxkv = sb(ks, "wxkv", [128, 8, 512], BF16)
            mT = sb(ks, "mT", [128, 8, MEM], BF16)
            xts = sbn(ks, "mxt", [128, D], F32, 2)
            xns = sbn(ks, "mxn", [128, D], BF16, 2)
            junk = sb(ks, "mjunk", [128, D], BF16)
            ssx = sb(ks, "mssx", [128, 2], F32)
            rsx = sb(ks, "mrsx", [128, 2], F32)
            load_gain(gMem, gvec["g_mem"], l)
            S.dma("pool", wxkv.t[:], w_view(w_xkv_d, l, 0, D), w=[wxkv.b])
            S.op("dve", lambda: dve.memset(vxa.t[:], 1.0), w=[vxa.b])
            for s in range(n_seq):
                for i in range(2):
                    xt = S.rot("mxt", xts)
                    S.dma("sp", xt.t[:], mem_in.ap()[s, i * 128:(i + 1) * 128, :], w=[xt.b])
                    rms_rstd(xt.t[:], D, junk, ssx.t[:, i:i + 1], ssx.b, rsx.t[:, i:i + 1], rsx.b, xt.b)
                    xn = S.rot("mxn", xns)
                    S.op("dve", lambda: dve.scalar_tensor_tensor(out=xn.t[:], in0=xt.t[:], scalar=rsx.t[:, i:i + 1], in1=gMem.t[:],
                                                                 op0=ALU.mult, op1=ALU.mult),
                         r=[xt.b, rsx.b, gMem.b], w=[xn.b])
                    transpose_to(mT.t[:, :, i * 128:(i + 1) * 128], mT.b, xn, 8)
                for cc in range(2):
                    pb = S.rot("psm", PS_M)
                    for c in range(8):
                        S.op("pe", lambda: pe.matmul(pb.t[:, 0:MEM], lhsT=wxkv.t[:, c, cc * 128:(cc + 1) * 128], rhs=mT.t[:, c, :],
                                                     start=(c == 0), stop=(c == 7)),
                             r=[wxkv.b, mT.b], w=[pb.b])
                    S.op("dve", lambda: dve.tensor_copy(out=kxT.t[:, s, cc, :], in_=pb.t[:, 0:MEM]), w=[pb.b, kxT.b])
                for i in range(2):
                    pb = S.rot("psm", PS_M)
                    for c in range(8):
                        S.op("pe", lambda: pe.matmul(pb.t[:, 0:256], lhsT=mT.t[:, c, i * 128:(i + 1) * 128], rhs=wxkv.t[:, c, 256:512],
                                                     start=(c == 0), stop=(c == 7)),
                             r=[wxkv.b, mT.b], w=[pb.b])
                    S.op("dve", lambda: dve.tensor_copy(out=vxa.t[:, s, i, :, 0:64],
                                                        in_=pb.t[:, 0:256].rearrange("p (h e) -> p h e", e=64)),
                         w=[pb.b, vxa.b])
            S.barrier()

        with contextlib.ExitStack() as ts:
            gFin = sb(ts, "gFin", [128, D], F32) if last else None
            gCT = sb(ts, "gCT", [128, 8], F32)
            gMT = sb(ts, "gMT", [128, 8], F32)
            wup = sb(ts, "wup", [128, 8, DFF], BF16)
            wdn = sb(ts, "wdn", [128, 32, D], BF16)
            wxq = sb(ts, "wxq", [128, 8, 256], BF16)
            wxo = sb(ts, "wxo", [128, 2, D], BF16)
            xgs = sbn(ts, "xg", [128, 2, D], F32, 2)
            xnk = sbn(ts, "txn", [128, D], BF16, 2)
            hTc = sb(ts, "hTc", [128, 8, 256], BF16)
            hTms = sbn(ts, "hTm", [128, 8, 256], BF16, 2)
            qxT = sb(ts, "qxT", [128, 4, 256], BF16)
            Eb = sbn(ts, "tEb", [128, 512], BF16, 2)
            oxn = sb(ts, "oxn", [128, 256], BF16)
            oxT = sb(ts, "oxT", [128, 2, 256], BF16)
            aT = sb(ts, "aT", [128, 32, 256], BF16)
            ssx = sb(ts, "tssx", [128, 2], F32)
            rsx = sb(ts, "trsx", [128, 2], F32)
            dden = sb(ts, "tdden", [128, 4], F32)
            rl = sbn(ts, "rl", [128, 256], F32, 2)
            S.op("pool", lambda: pool.memset(qxT.t[:], 0.0), w=[qxT.b])
            S.dma("sp", gCT.t[:], gT_in["gT_cross"].ap()[l], w=[gCT.b])
            S.dma("sp", gMT.t[:], gT_in["gT_mlp"].ap()[l], w=[gMT.b])
            if last:
                load_gain(gFin, gfin_in, 0)
            S.dma("pool", wxq.t[:], w_view(w_xq_d, l, 0, D), w=[wxq.b])
            S.dma("pool", wxo.t[:], w_view(w_xo_d, l, 0, 256), w=[wxo.b])
            for c4 in range(4):
                S.dma("pool", wup.t[:, :, c4 * 1024:(c4 + 1) * 1024],
                      w_up_d.ap()[l, :, c4 * 1024:(c4 + 1) * 1024].rearrange("(c p) n -> p c n", p=128), w=[wup.b])
            for c4 in range(4):
                S.dma("pool", wdn.t[:, c4 * 8:(c4 + 1) * 8, :], w_view(w_dn_d, l, c4 * 1024, 1024), w=[wdn.b])

            FS = [PS[0], PS[1]]
            FO = PS[2]
            UPB = [PS[4], PS[5]]
            DNB = [PS[3], PS[7]]
            groups = [(s, tg) for s in range(n_seq) for tg in range(SEQ // 256)]

            def norm_stage(xg, k, xn):
                def f():
                    S.op("act", lambda: act.activation(out=xn.t[:], in_=xg.t[:, k, :], func=AF.Square, accum_out=ssx.t[:, k:k + 1]),
                         r=[xg.b], w=[xn.b, ssx.b])
                    S.op("act", lambda: act.activation(out=rsx.t[:, k:k + 1], in_=ssx.t[:, k:k + 1], func=AF.Ln, scale=1.0 / D,
                                                       bias=epsc.t[:, 0:1]), r=[ssx.b, epsc.b], w=[rsx.b])
                    S.op("act", lambda: act.activation(out=rsx.t[:, k:k + 1], in_=rsx.t[:, k:k + 1], func=AF.Exp, scale=-0.5),
                         w=[rsx.b])
                    S.op("dve", lambda: dve.tensor_scalar(out=xn.t[:], in0=xg.t[:, k, :], scalar1=rsx.t[:, k:k + 1], scalar2=None,
                                                          op0=ALU.mult), r=[xg.b, rsx.b], w=[xn.b])
                return f

            def transp_stage(xn, dst, k, gT):
                def f():
                    pb = PS[6].t.bitcast(BF16)
                    for c in range(8):
                        S.op("pe", lambda: pe.transpose(out=pb[:, c * 128:(c + 1) * 128], in_=xn.t[:, c * 128:(c + 1) * 128],
                                                        identity=ident.t[:]), r=[xn.b, ident.b], w=[PS[6].b])
                    for c in range(8):
                        if c % 2 == 0 or _os.environ.get('T_NOACT'):
                            S.op("dve", lambda: dve.tensor_scalar(out=dst.t[:, c, k * 128:(k + 1) * 128], in0=pb[:, c * 128:(c + 1) * 128],
                                                                  scalar1=gT.t[:, c:c + 1], scalar2=None, op0=ALU.mult),
                                 r=[gT.b], w=[PS[6].b, dst.b])
                        else:
                            S.op("act", lambda: act.activation(out=dst.t[:, c, k * 128:(k + 1) * 128], in_=pb[:, c * 128:(c + 1) * 128],
                                                               func=AF.Identity, scale=gT.t[:, c:c + 1]),
                                 r=[gT.b], w=[PS[6].b, dst.b])
                return f

            def make_F(t):
                s, tg = groups[t]
                xg = xgs[t % 2]
                hTm = hTms[t % 2]
                st = []

                def load(k):
                    def f():
                        i = tg * 2 + k
                        S.dma("sp", xg.t[:, k, :], xA.ap()[s, i * 128:(i + 1) * 128, :], r=[xA_b[s][i]], w=[xg.b])
                    return f

                def qproj():
                    for cc in range(2):
                        for c in range(8):
                            S.op("pe", lambda: pe.matmul(FO.t[:, cc * 256:(cc + 1) * 256], lhsT=wxq.t[:, c, cc * 128:(cc + 1) * 128],
                                                         rhs=hTc.t[:, c, :], start=(c == 0 and cc == 0), stop=(c == 7),
                                                         skip_group_check=True),
                                 r=[wxq.b, hTc.b], w=[FO.b])
                    FOv = FO.t[:, :].rearrange("p (c t) -> p c t", t=256)
                    S.op("dve", lambda: dve.tensor_scalar(out=qxT.t[0:64, 0:4:2, :], in0=FOv[0:64, :, :],
                                                          scalar1=0.125, scalar2=None, op0=ALU.mult), w=[FO.b, qxT.b])
                    S.op("dve", lambda: dve.tensor_scalar(out=qxT.t[64:128, 1:4:2, :], in0=FOv[64:128, :, :],
                                                          scalar1=0.125, scalar2=None, op0=ALU.mult), w=[FO.b, qxT.b])

                def scores(k):
                    def f():
                        for idx in range(8):
                            h, mt = idx // 2, idx % 2
                            hp, hc = (h % 2) * 64, h // 2
                            bank = FS[h % 2]
                            col = ((h // 2) * 2 + mt) * 128
                            S.op("pe", lambda: pe.matmul(bank.t[:, col:col + 128], lhsT=kxT.t[:, s, hc, mt * 128:(mt + 1) * 128],
                                                         rhs=qxT.t[:, h, k * 128:(k + 1) * 128], start=True, stop=True,
                                                         skip_group_check=True),
                                 r=[kxT.b, qxT.b], w=[bank.b])
                        for bi in range(2):
                            S.op("act", lambda: act.activation(out=Eb[bi].t[:, :], in_=FS[bi].t[:, :], func=AF.Exp), w=[FS[bi].b, Eb[bi].b])
                    return f

                def pv(k):
                    def f():
                        first = True
                        for idx in range(8):
                            h, mt = idx // 2, idx % 2
                            e_ = Eb[h % 2]
                            col = ((h // 2) * 2 + mt) * 128
                            stf = first
                            S.op("pe", lambda: pe.matmul(FO.t[:, h * 65:(h + 1) * 65], lhsT=e_.t[:, col:col + 128], rhs=vxa.t[:, s, mt, h, :],
                                                         start=stf, stop=False, skip_group_check=True),
                                 r=[e_.b, vxa.b], w=[FO.b])
                            first = False
                        Ov = FO.t[:, 0:260].rearrange("p (j e) -> p j e", e=65)
                        S.op("dve", lambda: dve.reciprocal(out=dden.t[:], in_=Ov[:, :, 64]), w=[FO.b, dden.b])
                        for h in range(4):
                            S.op("dve", lambda: dve.tensor_scalar(out=oxn.t[:, h * 64:(h + 1) * 64], in0=Ov[:, h, 0:64],
                                                                  scalar1=dden.t[:, h:h + 1], scalar2=None, op0=ALU.mult),
                                 r=[dden.b], w=[FO.b, oxn.b])
                        pb = PS[6].t.bitcast(BF16)
                        for c in range(2):
                            S.op("pe", lambda: pe.transpose(out=pb[:, c * 128:(c + 1) * 128], in_=oxn.t[:, c * 128:(c + 1) * 128],
                                                            identity=ident.t[:]), r=[oxn.b, ident.b], w=[PS[6].b])
                        S.op("dve", lambda: dve.tensor_copy(out=oxT.t[:, :, k * 128:(k + 1) * 128],
                                                            in_=pb[:, 0:256].rearrange("p (c t) -> p c t", t=128)),
                             w=[PS[6].b, oxT.b])
                    return f

                def oproj(k):
                    def f():
                        for hf in range(2):
                            for c in range(2):
                                S.op("pe", lambda: pe.matmul(FO.t[:, :], lhsT=oxT.t[:, c, k * 128:(k + 1) * 128],
                                                             rhs=wxo.t[:, c, hf * 512:(hf + 1) * 512], start=(c == 0), stop=(c == 1)),
                                     r=[oxT.b, wxo.b], w=[FO.b])
                            S.op("dve", lambda: dve.tensor_tensor(out=xg.t[:, k, hf * 512:(hf + 1) * 512], in0=FO.t[:, :],
                                                                  in1=xg.t[:, k, hf * 512:(hf + 1) * 512], op=ALU.add),
                                 w=[FO.b, xg.b])
                    return f

                st.append(load(0))
                st.append(load(1))
                st.append(norm_stage(xg, 0, xnk[0]))
                st.append(norm_stage(xg, 1, xnk[1]))
                st.append(transp_stage(xnk[0], hTc, 0, gCT))
                st.append(transp_stage(xnk[1], hTc, 1, gCT))
                st.append(qproj)
                st.append(scores(0))
                st.append(pv(0))
                st.append(scores(1))
                st.append(pv(1))
                st.append(oproj(0))
                st.append(oproj(1))
                st.append(norm_stage(xg, 0, xnk[0]))
                st.append(norm_stage(xg, 1, xnk[1]))
                st.append(transp_stage(xnk[0], hTm, 0, gMT))
                st.append(transp_stage(xnk[1], hTm, 1, gMT))
                return st

            def make_M(t):
                s, tg = groups[t]
                xg = xgs[t % 2]
                hTm = hTms[t % 2]
                ch = []

                def up(fc):
                    def f():
                        pb = S.rot("upb", UPB)
                        for c in range(8):
                            S.op("pe", lambda: pe.matmul(pb.t[:, 0:256], lhsT=wup.t[:, c, fc * 128:(fc + 1) * 128], rhs=hTm.t[:, c, :],
                                                         start=(c == 0), stop=(c == 7)),
                                 r=[wup.b, hTm.b], w=[pb.b])
                        r_ = S.rot("rl", rl)
                        if fc % 2 == 0:
                            S.op("act", lambda: act.activation(out=r_.t[:, :], in_=pb.t[:, 0:256], func=AF.Relu), w=[pb.b, r_.b])
                            S.op("pool", lambda: pool.tensor_tensor(out=aT.t[:, fc, :], in0=r_.t[:, :], in1=r_.t[:, :], op=ALU.mult),
                                 r=[r_.b], w=[aT.b])
                        else:
                            S.op("dve", lambda: dve.tensor_scalar(out=r_.t[:, :], in0=pb.t[:, 0:256], scalar1=0.0, scalar2=None, op0=ALU.max),
                                 w=[pb.b, r_.b])
                            S.op("dve", lambda: dve.tensor_tensor(out=aT.t[:, fc, :], in0=r_.t[:, :], in1=r_.t[:, :], op=ALU.mult),
                                 r=[r_.b], w=[aT.b])
                    return f

                def down(k, hf):
                    def f():
                        pb = S.rot("dnb", DNB)
                        for fc in range(32):
                            S.op("pe", lambda: pe.matmul(pb.t[:, :], lhsT=aT.t[:, fc, k * 128:(k + 1) * 128],
                                                         rhs=wdn.t[:, fc, hf * 512:(hf + 1) * 512], start=(fc == 0), stop=(fc == 31)),
                                 r=[aT.b, wdn.b], w=[pb.b])
                        S.op("dve", lambda: dve.tensor_tensor(out=xg.t[:, k, hf * 512:(hf + 1) * 512], in0=pb.t[:, :],
                                                              in1=xg.t[:, k, hf * 512:(hf + 1) * 512], op=ALU.add),
                             w=[pb.b, xg.b])
                        if hf == 1:
                            i = tg * 2 + k
                            if last:
                                S.op("act", lambda: act.activation(out=xnk[k].t[:], in_=xg.t[:, k, :], func=AF.Square, accum_out=ssx.t[:, k:k + 1]),
                                     r=[xg.b], w=[xnk[k].b, ssx.b])
                                S.op("act", lambda: act.activation(out=rsx.t[:, k:k + 1], in_=ssx.t[:, k:k + 1], func=AF.Ln, scale=1.0 / D,
                                                                   bias=epsc.t[:, 0:1]), r=[ssx.b, epsc.b], w=[rsx.b])
                                S.op("act", lambda: act.activation(out=rsx.t[:, k:k + 1], in_=rsx.t[:, k:k + 1], func=AF.Exp, scale=-0.5),
                                     w=[rsx.b])
                                S.op("dve", lambda: dve.scalar_tensor_tensor(out=xg.t[:, k, :], in0=xg.t[:, k, :], scalar=rsx.t[:, k:k + 1],
                                                                             in1=gFin.t[:], op0=ALU.mult, op1=ALU.mult),
                                     r=[rsx.b, gFin.b], w=[xg.b])
                                S.dma("sp", y_out.ap()[s, i * 128:(i + 1) * 128, :], xg.t[:, k, :], r=[xg.b], w=[y_b[s][i]])
                            else:
                                S.dma("sp", xB.ap()[s, i * 128:(i + 1) * 128, :], xg.t[:, k, :], r=[xg.b], w=[xB_b[s][i]])
                    return f

                for fc in range(32):
                    ch.append(up(fc))
                for k in range(2):
                    for hf in range(2):
                        ch.append(down(k, hf))
                return ch

            _stop = int(_os.environ.get("T_STOP", "999"))
            for f in make_F(0)[:_stop]:
                f()
            for t in range(len(groups) if _stop >= 999 else 0):
                Mt = make_M(t)
                Fn = make_F(t + 1) if t + 1 < len(groups) else []
                sched = [0, 0, 1, 2, 5, 7, 9, 11, 13, 15, 17, 19, 20, 21, 22, 26, 28]
                fi = 0
                for ci, chunk in enumerate(Mt):
                    chunk()
                    while fi < len(Fn) and sched[fi] <= ci:
                        Fn[fi]()
                        fi += 1
                while fi < len(Fn):
                    Fn[fi]()
                    fi += 1
            S.barrier()
    S.barrier()
    return nc


_C_PERM = np.concatenate([np.arange(0, 64), np.arange(128, 192), np.arange(64, 128), np.arange(192, 256)])


def make_in_maps(inputs, n_cores, n_seq):
    f32 = np.float32
    w_in = np.asarray(inputs["w_in"], f32)
    perm = np.arange(IN_W)
    perm[C_Q:C_Q + 256] = C_Q + _C_PERM
    shared = {
        "rel_table": np.ascontiguousarray(inputs["rel_table"], f32),
        "g_final": np.asarray(inputs["g_final"], f32).reshape(1, D),
        "sinks": np.asarray(inputs["sinks"], f32).reshape(1, DEPTH * 4),
        "w_in": np.ascontiguousarray(w_in[:, :, perm]),
    }
    for k in ("g_mix", "g_group", "g_cross", "g_mem", "g_mlp", "w_out", "w_xq", "w_xkv", "w_xo", "w_up", "w_down"):
        shared[k] = np.ascontiguousarray(inputs[k], f32)
    for k, src in (("gT_cross", "g_cross"), ("gT_mlp", "g_mlp")):
        shared[k] = np.ascontiguousarray(np.asarray(inputs[src], f32).reshape(DEPTH, 8, 128).transpose(0, 2, 1))
    shared.update(_host_consts())
    x = np.asarray(inputs["x"], f32)
    mem = np.asarray(inputs["mem"], f32)
    maps = []
    for c in range(n_cores):
        m = dict(shared)
        m["x"] = np.ascontiguousarray(x[c * n_seq:(c + 1) * n_seq])
        m["mem"] = np.ascontiguousarray(mem[c * n_seq:(c + 1) * n_seq])
        maps.append(m)
    return maps


def kernel(**inputs):
    B = inputs["x"].shape[0]
    n_seq = B // N_CORES
    nc = build_program(n_seq)
    maps = make_in_maps(inputs, N_CORES, n_seq)
    res = run_bass_kernel_spmd(nc, maps, core_ids=list(range(N_CORES)))
    return np.concatenate([np.asarray(r["y"], np.float32) for r in res.results], axis=0)
```

```python
import math
import os as _os
import contextlib
import numpy as np
import ml_dtypes
import concourse.bass as bass
import concourse.mybir as mybir
from concourse.bass_utils import run_bass_kernel_spmd

F32 = mybir.dt.float32
BF16 = mybir.dt.bfloat16
AF = mybir.ActivationFunctionType
ALU = mybir.AluOpType

D = 1024
SEQ = 2048
NT = SEQ // 128
MEM = 256
DEPTH = 2
IN_W = 2816
DFF = 4096
NDIST = SEQ + 127
EPS = 1e-6
N_CORES = 8
BIG = 1.0e30

A_Q, A_K, A_V = 0, 256, 512
B_Q, B_K, B_V = 768, 1024, 1280
C_Q, C_K, C_V = 1536, 1792, 1920
D_Q, D_K, D_V = 2048, 2304, 2560


class Buf:
    __slots__ = ("name", "lw", "rd")

    def __init__(self, name):
        self.name = name
        self.lw = None
        self.rd = {}


class Tile:
    def __init__(self, t, b):
        self.t = t
        self.b = b


class Sched:
    EPOCH = 16000

    def __init__(self, nc, es):
        self.nc = nc
        self.es = es
        self.eng = {"pe": nc.tensor, "act": nc.scalar, "dve": nc.vector, "pool": nc.gpsimd, "sp": nc.sync}
        self.cnt = {k: 0 for k in self.eng}
        self.sems = {k: [] for k in self.eng}
        self.seen = {k: {} for k in self.eng}
        self.seen_dma = {k: set() for k in self.eng}
        self.q = {}
        self.rr = {}

    def newsem(self, name):
        return self.es.enter_context(self.nc.semaphore(name))

    def add_queue(self, name, issuer, K=8):
        self.q[name] = dict(issuer=issuer, sems=[self.newsem(f"dq_{name}_{i}") for i in range(K)], n=0, K=K)

    def _sem_for(self, e, c):
        idx = (c - 1) // self.EPOCH
        while len(self.sems[e]) <= idx:
            self.sems[e].append(self.newsem(f"s_{e}_{len(self.sems[e])}"))
        return self.sems[e][idx], c - idx * self.EPOCH

    def _wait(self, e, tok):
        if tok[0] == "e":
            _, x, c = tok
            if e == "pe" and x == "pe":
                return
            if self.seen[e].get(x, 0) >= c:
                return
            sem, v = self._sem_for(x, c)
            self.eng[e].wait_ge(sem, v)
            self.seen[e][x] = c
        else:
            _, qn, i = tok
            if tok in self.seen_dma[e]:
                return
            Q = self.q[qn]
            self.eng[e].wait_ge(Q["sems"][i % Q["K"]], 16 * (i // Q["K"] + 1))
            self.seen_dma[e].add(tok)

    @staticmethod
    def _deps(r, w):
        toks = []
        for b in r:
            if b.lw is not None:
                toks.append(b.lw)
        for b in w:
            if b.lw is not None:
                toks.append(b.lw)
            toks.extend(b.rd.values())
        return toks

    @staticmethod
    def _commit(tok, key, r, w):
        for b in w:
            b.lw = tok
            b.rd = {}
        for b in r:
            if b not in w:
                b.rd[key] = tok

    def op(self, e, fn, r=(), w=()):
        for t in self._deps(r, w):
            self._wait(e, t)
        inst = fn()
        self.cnt[e] += 1
        c = self.cnt[e]
        sem, _ = self._sem_for(e, c)
        inst.then_inc(sem, 1)
        self._commit(("e", e, c), e, r, w)

    def dma(self, qn, out, in_, r=(), w=()):
        Q = self.q[qn]
        e = Q["issuer"]
        i = Q["n"]
        if i >= Q["K"]:
            self._wait(e, ("d", qn, i - Q["K"]))
        for t in self._deps(r, w):
            self._wait(e, t)
        inst = self.eng[e].dma_start(out=out, in_=in_)
        inst.then_inc(Q["sems"][i % Q["K"]], 16)
        Q["n"] += 1
        tok = ("d", qn, i)
        self._commit(tok, tok, r, w)

    def barrier(self):
        for e in self.eng:
            for x in self.eng:
                if x != e and self.cnt[x] > 0:
                    self._wait(e, ("e", x, self.cnt[x]))
            for qn, Q in self.q.items():
                for i in range(max(0, Q["n"] - Q["K"]), Q["n"]):
                    self._wait(e, ("d", qn, i))

    def rot(self, key, lst):
        i = self.rr.get(key, 0)
        self.rr[key] = i + 1
        return lst[i % len(lst)]


def _rel_bucket_np(dist):
    max_exact = 16
    n = np.maximum(dist, 0)
    nf = np.maximum(n, 1).astype(np.float32)
    large = max_exact + (np.log(nf / np.float32(max_exact)) / np.float32(math.log(2048 / max_exact))
                         * np.float32(32 - max_exact)).astype(np.int32)
    large = np.minimum(large, 31)
    return np.where(n < max_exact, n, large)


def _host_consts():
    bf = ml_dtypes.bfloat16
    idx = np.arange(128)
    ident = np.eye(128, dtype=np.float32)
    jrev = ident[::-1].copy()
    trineg = -(idx[:, None] >= idx[None, :]).astype(np.float32)
    maskb = (idx[:, None] < idx[None, :]).astype(np.float32)
    dist = np.arange(NDIST) - 127
    bucket = _rel_bucket_np(dist)
    oh = np.zeros((32, NDIST), np.float32)
    oh[bucket, np.arange(NDIST)] = 1.0
    mult = np.zeros((12, NDIST), np.float32)
    ge0 = dist >= 0
    ma = ((dist <= 128) & ge0).astype(np.float32) + ((dist % 4 == 0) & (dist <= 512) & ge0) + ((dist % 16 == 0) & ge0)
    mult[0:4] = ma
    mult[4:8] = (ge0 & (dist <= 127)).astype(np.float32)
    mult[8:12] = ge0.astype(np.float32)
    return {
        "c_ident": ident.astype(bf),
        "c_jrev": jrev,
        "c_trineg": trineg.astype(bf),
        "c_maskb": maskb.astype(bf),
        "c_oh": oh,
        "c_mult": mult,
    }


def build_program(n_seq, n_layers=DEPTH, dbg=False):
    nc = bass.Bass("TRN2", target_bir_lowering=False)
    es = contextlib.ExitStack()
    S = Sched(nc, es)
    S.add_queue("sp", "sp", K=8)
    S.add_queue("pool", "pool", K=4)

    def dram(name, shape, dt, kind):
        return nc.dram_tensor(name, list(shape), dt, kind=kind)

    x_in = dram("x", [n_seq, SEQ, D], F32, "ExternalInput")
    mem_in = dram("mem", [n_seq, MEM, D], F32, "ExternalInput")
    rel_in = dram("rel_table", [32, 12], F32, "ExternalInput")
    gvec = {k: dram(k, [DEPTH, D], F32, "ExternalInput") for k in ("g_mix", "g_group", "g_cross", "g_mem", "g_mlp")}
    gfin_in = dram("g_final", [1, D], F32, "ExternalInput")
    gT_in = {k: dram(k, [DEPTH, 128, 8], F32, "ExternalInput") for k in ("gT_cross", "gT_mlp")}
    sinks_in = dram("sinks", [1, DEPTH * 4], F32, "ExternalInput")
    w_in_d = dram("w_in", [DEPTH, D, IN_W], F32, "ExternalInput")
    w_out_d = dram("w_out", [DEPTH, D, D], F32, "ExternalInput")
    w_xq_d = dram("w_xq", [DEPTH, D, 256], F32, "ExternalInput")
    w_xkv_d = dram("w_xkv", [DEPTH, D, 512], F32, "ExternalInput")
    w_xo_d = dram("w_xo", [DEPTH, 256, D], F32, "ExternalInput")
    w_up_d = dram("w_up", [DEPTH, D, DFF], F32, "ExternalInput")
    w_dn_d = dram("w_down", [DEPTH, DFF, D], F32, "ExternalInput")
    c_ident = dram("c_ident", [128, 128], BF16, "ExternalInput")
    c_jrev = dram("c_jrev", [128, 128], F32, "ExternalInput")
    c_trineg = dram("c_trineg", [128, 128], BF16, "ExternalInput")
    c_maskb = dram("c_maskb", [128, 128], BF16, "ExternalInput")
    c_oh = dram("c_oh", [32, NDIST], F32, "ExternalInput")
    c_mult = dram("c_mult", [12, NDIST], F32, "ExternalInput")
    y_out = dram("y", [n_seq, SEQ, D], F32, "ExternalOutput")
    xA = dram("xA", [n_seq, SEQ, D], F32, "Internal")
    xB = dram("xB", [n_seq, SEQ, D], F32, "Internal")
    Fd = dram("Fd", [12, NDIST + 1], F32, "Internal")
    Texp = dram("Texp", [12, 128, SEQ], BF16, "Internal")
    dbg_out = {}
    if dbg:
        dbg_out["x1"] = dram("dbg_x1", [n_seq, SEQ, D], F32, "ExternalOutput")
        dbg_out["yg"] = dram("dbg_yg", [4, SEQ, 256], F32, "ExternalOutput")

    def dbuf(name):
        return [[Buf(f"{name}_{s}_{i}") for i in range(NT)] for s in range(n_seq)]

    xA_b, xB_b, y_b = dbuf("xA"), dbuf("xB"), dbuf("y")
    Texp_b = [Buf(f"Texp{h}") for h in range(12)]
    none_b = []

    uid = [0]

    def sb(stack, name, shape, dt):
        uid[0] += 1
        name = f"{name}_u{uid[0]}"
        t = stack.enter_context(nc.sbuf_tensor(name, list(shape), dt))
        return Tile(t, Buf(name))

    def sbn(stack, name, shape, dt, n):
        return [sb(stack, f"{name}{i}", shape, dt) for i in range(n)]

    PS = []
    for i in range(8):
        t = es.enter_context(nc.psum_tensor(f"ps{i}", [128, 512], F32))
        PS.append(Tile(t, Buf(f"ps{i}")))
    PS_S = [PS[0], PS[1]]
    PS_O = [PS[2], PS[3]]
    PS_M = [PS[4], PS[5]]
    PS_T = PS[6]
    PS_X = PS[7]

    ident = sb(es, "ident", [128, 128], BF16)
    trineg = sb(es, "trineg", [128, 128], BF16)
    maskb = sb(es, "maskb", [128, 128], BF16)
    onescol = sb(es, "onescol", [128, 2], BF16)
    expsink = sb(es, "expsink", [128, DEPTH * 4], F32)
    epsc = sb(es, "epsc", [128, 1], F32)

    act = nc.scalar
    dve = nc.vector
    pool = nc.gpsimd
    pe = nc.tensor

    with contextlib.ExitStack() as ps_:
        S.dma("sp", ident.t[:], c_ident.ap(), w=[ident.b])
        S.dma("sp", trineg.t[:], c_trineg.ap(), w=[trineg.b])
        S.dma("sp", maskb.t[:], c_maskb.ap(), w=[maskb.b])
        S.op("dve", lambda: dve.memset(onescol.t[:], 1.0), w=[onescol.b])
        S.op("dve", lambda: dve.memset(epsc.t[:], EPS), w=[epsc.b])
        S.dma("sp", expsink.t[:], sinks_in.ap()[0:1, :].partition_broadcast(128), w=[expsink.b])
        S.op("act", lambda: act.activation(out=expsink.t[:], in_=expsink.t[:], func=AF.Exp), r=[expsink.b], w=[expsink.b])

        tab = sb(ps_, "tab", [32, 12], F32)
        oh = sb(ps_, "oh", [32, NDIST], F32)
        mu = sb(ps_, "mu", [12, NDIST], F32)
        Fs = sb(ps_, "Fs", [12, NDIST + 1], F32)
        jrev = sb(ps_, "jrev", [128, 128], F32)
        Xs = sbn(ps_, "Xs", [128, 512], F32, 2)
        Tb = sbn(ps_, "Tb", [128, 512], BF16, 2)
        S.dma("sp", tab.t[:], rel_in.ap(), w=[tab.b])
        S.dma("sp", oh.t[:], c_oh.ap(), w=[oh.b])
        S.dma("sp", mu.t[:], c_mult.ap(), w=[mu.b])
        S.dma("sp", jrev.t[:], c_jrev.ap(), w=[jrev.b])
        S.op("dve", lambda: dve.memset(Fs.t[:], 0.0), w=[Fs.b])
        for ch in range((NDIST + 511) // 512):
            c0 = ch * 512
            n = min(512, NDIST - c0)
            pb = S.rot("psm", PS_M)
            S.op("pe", lambda: pe.matmul(pb.t[0:12, 0:n], lhsT=tab.t[:, :], rhs=oh.t[:, c0:c0 + n], start=True, stop=True),
                 r=[tab.b, oh.b], w=[pb.b])
            S.op("act", lambda: act.activation(out=Fs.t[:, c0:c0 + n], in_=pb.t[0:12, 0:n], func=AF.Exp), w=[pb.b, Fs.b])
            S.op("dve", lambda: dve.tensor_tensor(out=Fs.t[:, c0:c0 + n], in0=Fs.t[:, c0:c0 + n], in1=mu.t[:, c0:c0 + n], op=ALU.mult),
                 r=[mu.b], w=[Fs.b])
        Fd_b = Buf("Fd")
        S.dma("sp", Fd.ap(), Fs.t[:], r=[Fs.b], w=[Fd_b])
        for rh in range(12):
            W = 256 if 4 <= rh < 8 else SEQ
            for c0 in range(0, W, 512):
                n = min(512, W - c0)
                X = S.rot("Xs", Xs)
                T_ = S.rot("Tb", Tb)
                src = bass.AP(Fd, rh * (NDIST + 1) + c0, [[1, 128], [1, n]])
                S.dma("sp", X.t[:, 0:n], src, r=[Fd_b], w=[X.b])
                pb = S.rot("psm", PS_M)
                S.op("pe", lambda: pe.matmul(pb.t[:, 0:n], lhsT=jrev.t[:], rhs=X.t[:, 0:n], start=True, stop=True),
                     r=[jrev.b, X.b], w=[pb.b])
                S.op("dve", lambda: dve.tensor_copy(out=T_.t[:, 0:n], in_=pb.t[:, 0:n]), w=[pb.b, T_.b])
                S.dma("sp", Texp.ap()[rh, :, c0:c0 + n], T_.t[:, 0:n], r=[T_.b], w=[Texp_b[rh]])
        S.barrier()

    def load_gain(tile_, src_handle, row):
        S.dma("sp", tile_.t[:], src_handle.ap()[row:row + 1, :].partition_broadcast(128), w=[tile_.b])

    def w_view(handle, l, rows0, nrows):
        return handle.ap()[l, rows0:rows0 + nrows, :].rearrange("(c p) n -> p c n", p=128)

    def rms_rstd(x_ap, ncols, junk, ss_t, ss_b, rstd_t, rstd_b, xb):
        S.op("act", lambda: act.activation(out=junk.t[:, 0:ncols], in_=x_ap, func=AF.Square, accum_out=ss_t),
             r=[xb], w=[junk.b, ss_b])
        S.op("act", lambda: act.activation(out=rstd_t, in_=ss_t, func=AF.Ln, scale=1.0 / ncols, bias=epsc.t[:, 0:1]),
             r=[ss_b, epsc.b], w=[rstd_b])
        S.op("act", lambda: act.activation(out=rstd_t, in_=rstd_t, func=AF.Exp, scale=-0.5), r=[rstd_b], w=[rstd_b])

    def transpose_to(dst_ap3, dst_b, src_tile, nchunks, eng="dve"):
        pb = PS_T.t.bitcast(BF16)
        for c in range(nchunks):
            S.op("pe", lambda: pe.transpose(out=pb[:, c * 128:(c + 1) * 128], in_=src_tile.t[:, c * 128:(c + 1) * 128],
                                            identity=ident.t[:]),
                 r=[src_tile.b, ident.b], w=[PS_T.b])
        srcv = pb[:, 0:nchunks * 128].rearrange("p (k t) -> p k t", t=128)
        if eng == "dve":
            S.op("dve", lambda: dve.tensor_copy(out=dst_ap3, in_=srcv), w=[PS_T.b, dst_b])
        else:
            S.op("act", lambda: act.copy(out=dst_ap3, in_=srcv), w=[PS_T.b, dst_b])

    kxT = sb(es, "kxT", [128, n_seq, 2, MEM], BF16)
    vxa = sb(es, "vxa", [128, n_seq, 2, 4, 65], BF16)
    for l in range(n_layers):
        x_src, x_src_b = (x_in, None) if l == 0 else (xB, xB_b)
        last = l == n_layers - 1

        with contextlib.ExitStack() as ms:
            gMix = sb(ms, "gMix", [128, D], F32)
            gGrp = sb(ms, "gGrp", [128, D], F32)
            hT = sb(ms, "hT", [128, 8, SEQ], BF16)
            wsl = sb(ms, "wsl", [128, 8, 768], BF16)
            wout = sb(ms, "wout", [128, 8, D], BF16)
            Ttab = sb(ms, "Ttab", [128, 4, SEQ], BF16)
            TtabC = sb(ms, "TtabC", [128, 4, 256], BF16)
            qT = sb(ms, "qT", [128, 4, SEQ], BF16)
            kT = sb(ms, "kT", [128, 2, SEQ], BF16)
            vaug = sb(ms, "vaug", [128, NT, 4, 65], BF16)
            yg = sb(ms, "yg", [128, NT, 256], F32)
            yT = sb(ms, "yT", [128, 8, SEQ], BF16)
            xts = sbn(ms, "xt", [128, D], F32, 2)
            xns = sbn(ms, "xn", [128, D], BF16, 2)
            junk = sb(ms, "junk", [128, D], BF16)
            Ebufs = sbn(ms, "Eb", [128, 512], BF16, 3)
            Pbufs = sbn(ms, "Pb", [128, 512], BF16, 4)
            Ubufs = sbn(ms, "Ub", [128, 512], F32, 2)
            Wbufs = sbn(ms, "Wb", [128, 512], BF16, 3)
            tmpc = sb(ms, "tmpc", [128, 4, 65], F32)
            Osb = sb(ms, "Osb", [128, 4, 65], F32)
            ssx = sb(ms, "ssx", [128, 2], F32)
            rsx = sb(ms, "rsx", [128, 2], F32)
            ssg = sb(ms, "ssg", [128, NT], F32)
            rsg = sb(ms, "rsg", [128, NT], F32)
            dden = sb(ms, "dden", [128, 4], F32)
            Rb = sb(ms, "Rb", [128, 4], F32)
            Cbs = sbn(ms, "Cb", [128, 4], F32, 2)
            gate = sb(ms, "gate", [128, 16], F32)
            top8 = sb(ms, "top8", [128, 8], F32)
            Msel = sb(ms, "Msel", [128, 4, NT, 8], F32)
            ksum = sb(ms, "ksum", [128, 2, 8], F32)
            ksumb = sb(ms, "ksumb", [128, 2, 8], BF16)

            load_gain(gMix, gvec["g_mix"], l)
            load_gain(gGrp, gvec["g_group"], l)
            S.dma("pool", wout.t[:], w_view(w_out_d, l, 0, D), w=[wout.b])
            S.dma("sp", TtabC.t[:], Texp.ap()[4:8, :, 0:256].rearrange("h p t -> p h t"), r=Texp_b[4:8], w=[TtabC.b])
            S.op("dve", lambda: dve.memset(vaug.t[:], 1.0), w=[vaug.b])
            S.op("pool", lambda: pool.memset(qT.t[:], 0.0), w=[qT.b])

            def load_wslice(col0, ncols):
                S.dma("pool", wsl.t[:, :, 0:ncols],
                      w_in_d.ap()[l, :, col0:col0 + ncols].rearrange("(c p) n -> p c n", p=128), w=[wsl.b])

            def proj_T(dst, nchunk, wcol0, scale, ei):
                return [(lambda cc=cc, tg=tg: proj_T_chunk(dst, cc, tg, wcol0, scale, ei)) for cc in range(nchunk) for tg in range(4)]

            def proj_T_chunk(dst, cc, tg, wcol0, scale, ei):
                if True:
                    if True:
                        pb = S.rot("psm", PS_M)
                        for c in range(8):
                            S.op("pe", lambda: pe.matmul(pb.t[:, :], lhsT=wsl.t[:, c, wcol0 + cc * 128: wcol0 + (cc + 1) * 128],
                                                         rhs=hT.t[:, c, tg * 512:(tg + 1) * 512], start=(c == 0), stop=(c == 7)),
                                 r=[wsl.b, hT.b], w=[pb.b])
                        ei[0] += 1
                        if dst is qT:
                            S.op("dve", lambda: dve.tensor_scalar(out=qT.t[0:64, 2 * cc, tg * 512:(tg + 1) * 512], in0=pb.t[0:64, :],
                                                                  scalar1=scale, scalar2=None, op0=ALU.mult),
                                 w=[pb.b, dst.b])
                            S.op("act", lambda: act.activation(out=qT.t[64:128, 2 * cc + 1, tg * 512:(tg + 1) * 512], in_=pb.t[64:128, :],
                                                               func=AF.Copy, scale=scale), w=[pb.b, dst.b])
                        elif ei[0] % 2 == 0:
                            S.op("dve", lambda: dve.tensor_scalar(out=dst.t[:, cc, tg * 512:(tg + 1) * 512], in0=pb.t[:, :],
                                                                  scalar1=scale, scalar2=None, op0=ALU.mult),
                                 w=[pb.b, dst.b])
                        else:
                            S.op("act", lambda: act.activation(out=dst.t[:, cc, tg * 512:(tg + 1) * 512], in_=pb.t[:, :],
                                                               func=AF.Copy, scale=scale), w=[pb.b, dst.b])

            def proj_V(wcol0, nh, ei):
                return [(lambda i=i: proj_V_chunk(wcol0, nh, ei, i)) for i in range(NT)]

            def proj_V_chunk(wcol0, nh, ei, i):
                if True:
                    pb = S.rot("psm", PS_M)
                    for c in range(8):
                        S.op("pe", lambda: pe.matmul(pb.t[:, 0:nh * 64], lhsT=hT.t[:, c, i * 128:(i + 1) * 128],
                                                     rhs=wsl.t[:, c, wcol0:wcol0 + nh * 64], start=(c == 0), stop=(c == 7)),
                             r=[wsl.b, hT.b], w=[pb.b])
                    srcv = pb.t[:, 0:nh * 64].rearrange("p (h e) -> p h e", e=64)
                    ei[0] += 1
                    if ei[0] % 2 == 0:
                        S.op("dve", lambda: dve.tensor_copy(out=vaug.t[:, i, 0:nh, 0:64], in_=srcv), w=[pb.b, vaug.b])
                    else:
                        S.op("act", lambda: act.copy(out=vaug.t[:, i, 0:nh, 0:64], in_=srcv), w=[pb.b, vaug.b])

            mulrr = [0]

            def mul_T(out_ap, in0_ap, in1_ap, r, w):
                mulrr[0] += 1
                if mulrr[0] % 2 == 0:
                    S.op("dve", lambda: dve.tensor_tensor(out=out_ap, in0=in0_ap, in1=in1_ap, op=ALU.mult), r=r, w=w)
                else:
                    S.op("pool", lambda: pool.tensor_tensor(out=out_ap, in0=in0_ap, in1=in1_ap, op=ALU.mult), r=r, w=w)

            def finish_group(g, Ob, h, extra=None, add_osb=False):
                Ov = Ob.t[:, 0:260].rearrange("p (j e) -> p j e", e=65)
                if add_osb:
                    S.op("dve", lambda: dve.tensor_tensor(out=Osb.t[:], in0=Ov, in1=Osb.t[:], op=ALU.add), w=[Ob.b, Osb.b])
                    src, srcb = Osb.t, Osb.b
                    den_ap = Osb.t[:, :, 64]
                else:
                    src, srcb = None, Ob.b
                    den_ap = Ov[:, :, 64]
                if extra is not None:
                    S.op("dve", lambda: dve.tensor_scalar(out=dden.t[:], in0=den_ap, scalar1=extra, scalar2=None, op0=ALU.add),
                         r=[expsink.b], w=[srcb, dden.b])
                    S.op("dve", lambda: dve.reciprocal(out=dden.t[:], in_=dden.t[:]), w=[dden.b])
                else:
                    S.op("dve", lambda: dve.reciprocal(out=dden.t[:], in_=den_ap), w=[srcb, dden.b])
                sap = Osb.t[:, :, 0:64] if add_osb else Ov[:, :, 0:64]
                dbc = bass.AP(dden.t, 0, [[4, 128], [1, 4], [0, 64]])
                S.op("dve", lambda: dve.tensor_tensor(out=yg.t[:, 4 * g:4 * g + 4, h * 64:(h + 1) * 64], in0=sap, in1=dbc, op=ALU.mult),
                     r=[dden.b], w=[srcb, yg.b])

            SB3 = [PS[0], PS[1], PS[4]]
            OB2 = [PS[2], PS[3]]
            PVB = [PS[5], PS[7]]

            def run_pipeline(steps, lags):
                n = len(steps)
                offs = [0]
                for lg in lags:
                    offs.append(offs[-1] + lg)
                for t in range(n + offs[-1]):
                    for si, o in enumerate(offs):
                        k = t - o
                        if 0 <= k < n:
                            steps[k][si]()

            def softmax_steps(h, kp, kc, qp, qc, vh, Tap, Tb_, window, extra=None):
                steps = []
                for g in range(4):
                    grp = {"Ob": None, "first": True}
                    bl = []
                    for b in range(max(0, 4 * g - window), 4 * g + 4):
                        jlo = max(0, b - 4 * g)
                        jhi = min(3, b + window - 4 * g)
                        if jlo <= jhi:
                            bl.append((b, jlo, jhi))
                    for idx, (b, jlo, jhi) in enumerate(bl):
                        st = {}
                        last = idx == len(bl) - 1

                        def F(st=st, b=b, jlo=jlo, jhi=jhi, g=g):
                            ncol = (jhi - jlo + 1) * 128
                            q0 = (4 * g + jlo) * 128
                            Sb_ = S.rot("sb3", SB3)
                            S.op("pe", lambda: pe.matmul(Sb_.t[:, 0:ncol], lhsT=kT.t[:, kc, b * 128:(b + 1) * 128],
                                                         rhs=qT.t[:, qc, q0:q0 + ncol], start=True, stop=True),
                                 r=[kT.b, qT.b], w=[Sb_.b])
                            e_ = S.rot("eb", Ebufs)
                            S.op("act", lambda: act.activation(out=e_.t[:, 0:ncol], in_=Sb_.t[:, 0:ncol], func=AF.Exp), w=[Sb_.b, e_.b])
                            p_ = S.rot("pb", Pbufs)
                            tau0 = q0 - 128 * b
                            S.op("dve", lambda: dve.tensor_tensor(out=p_.t[:, 0:ncol], in0=e_.t[:, 0:ncol], in1=Tap[:, tau0:tau0 + ncol], op=ALU.mult),
                                 r=[e_.b, Tb_], w=[p_.b])
                            st["p"] = p_

                        def B(st=st, b=b, jlo=jlo, jhi=jhi, g=g, grp=grp, last=last):
                            if grp["Ob"] is None:
                                grp["Ob"] = S.rot("ob2", OB2)
                            Ob = grp["Ob"]
                            p_ = st["p"]
                            for j in range(jlo, jhi + 1):
                                stf = grp["first"]
                                S.op("pe", lambda: pe.matmul(Ob.t[:, j * 65:(j + 1) * 65], lhsT=p_.t[:, (j - jlo) * 128:(j - jlo + 1) * 128],
                                                             rhs=vaug.t[:, b, vh, :], start=stf, stop=False, skip_group_check=True),
                                     r=[p_.b, vaug.b], w=[Ob.b])
                                grp["first"] = False
                            if last:
                                finish_group(g, Ob, h, extra=extra)

                        steps.append((F, B))
                return steps

            def stick_steps(h, kp, kc):
                steps = []
                for g in range(4):
                    for b in range(4 * g + 3, -1, -1):
                        st = {}
                        firstg = b == 4 * g + 3
                        lastg = b == 0

                        def S1(st=st, b=b, g=g):
                            jlo = max(0, b - 4 * g)
                            ncol = (4 - jlo) * 128
                            q0 = (4 * g + jlo) * 128
                            diag = b >= 4 * g
                            Zb = S.rot("sb3", SB3)
                            S.op("pe", lambda: pe.matmul(Zb.t[:, 0:ncol], lhsT=kT.t[:, kc, b * 128:(b + 1) * 128],
                                                         rhs=qT.t[:, h, q0:q0 + ncol], start=True, stop=False,
                                                         skip_group_check=True),
                                 r=[kT.b, qT.b], w=[Zb.b])
                            u_ = S.rot("ub", Ubufs)
                            S.op("act", lambda: act.activation(out=u_.t[:, 0:ncol], in_=Zb.t[:, 0:ncol], func=AF.Exp), w=[Zb.b, u_.b])
                            w_ = S.rot("wb", Wbufs)
                            S.op("act", lambda: act.activation(out=w_.t[:, 0:ncol], in_=u_.t[:, 0:ncol], func=AF.Ln, bias=1.0),
                                 r=[u_.b], w=[w_.b])
                            if diag:
                                S.op("pool", lambda: pool.tensor_tensor(out=w_.t[:, 0:128], in0=w_.t[:, 0:128], in1=maskb.t[:], op=ALU.mult),
                                     r=[maskb.b], w=[w_.b])
                            st.update(Zb=Zb, w=w_, jlo=jlo, ncol=ncol, diag=diag)

                        def S2(st=st):
                            Zb, w_, ncol = st["Zb"], st["w"], st["ncol"]
                            S.op("pe", lambda: pe.matmul(Zb.t[:, 0:ncol], lhsT=trineg.t[:], rhs=w_.t[:, 0:ncol], start=False, stop=True,
                                                         skip_group_check=True),
                                 r=[trineg.b, w_.b], w=[Zb.b])
                            p_ = S.rot("pb", Pbufs)
                            S.op("act", lambda: act.activation(out=p_.t[:, 0:ncol], in_=Zb.t[:, 0:ncol], func=AF.Exp), w=[Zb.b, p_.b])
                            if st["diag"]:
                                S.op("pool", lambda: pool.tensor_tensor(out=p_.t[:, 0:128], in0=p_.t[:, 0:128], in1=maskb.t[:], op=ALU.mult),
                                     r=[maskb.b], w=[p_.b])
                            st["p"] = p_

                        def S3(st=st, b=b, g=g, firstg=firstg, lastg=lastg):
                            p_, w_, jlo = st["p"], st["w"], st["jlo"]
                            if firstg:
                                S.op("dve", lambda: dve.memset(Osb.t[:], 0.0), w=[Osb.b])
                                S.op("dve", lambda: dve.memset(Rb.t[:], 0.0), w=[Rb.b])
                                S.op("dve", lambda: dve.memset(Cbs[0].t[:], 1.0), w=[Cbs[0].b])
                                S.op("dve", lambda: dve.memset(Cbs[1].t[:], 1.0), w=[Cbs[1].b])
                            kk = (4 * g + 3 - b) % 2
                            Cc, Cn = Cbs[kk], Cbs[1 - kk]
                            Pv = S.rot("pvb", PVB)
                            first = True
                            for j in range(jlo, 4):
                                stf = first
                                S.op("pe", lambda: pe.matmul(Pv.t[:, j * 64:(j + 1) * 64], lhsT=p_.t[:, (j - jlo) * 128:(j - jlo + 1) * 128],
                                                             rhs=vaug.t[:, b, h, 0:64], start=stf, stop=False, skip_group_check=True),
                                     r=[p_.b, vaug.b], w=[Pv.b])
                                first = False
                                if not lastg:
                                    S.op("pe", lambda: pe.matmul(Pv.t[:, 256 + j:257 + j], lhsT=w_.t[:, (j - jlo) * 128:(j - jlo + 1) * 128],
                                                                 rhs=onescol.t[:, 0:1], start=False, stop=False, skip_group_check=True),
                                         r=[w_.b, onescol.b], w=[Pv.b])
                            if not lastg:
                                S.op("dve", lambda: dve.tensor_tensor(out=Rb.t[:, jlo:4], in0=Pv.t[:, 256 + jlo:260], in1=Rb.t[:, jlo:4], op=ALU.add),
                                     w=[Pv.b, Rb.b])
                                S.op("act", lambda: act.activation(out=Cn.t[:, jlo:4], in_=Rb.t[:, jlo:4], func=AF.Exp, scale=-1.0),
                                     r=[Rb.b], w=[Cn.b])
                            nj = 4 - jlo
                            cbc = bass.AP(Cc.t, jlo, [[4, 128], [1, nj], [0, 64]])
                            Pv3 = Pv.t[:, jlo * 64:256].rearrange("p (j e) -> p j e", e=64)
                            S.op("dve", lambda: dve.tensor_tensor(out=tmpc.t[:, jlo:4, 0:64], in0=Pv3, in1=cbc, op=ALU.mult),
                                 r=[Cc.b], w=[Pv.b, tmpc.b])
                            S.op("dve", lambda: dve.tensor_tensor(out=Osb.t[:, jlo:4, 0:64], in0=tmpc.t[:, jlo:4, 0:64], in1=Osb.t[:, jlo:4, 0:64],
                                                                  op=ALU.add),
                                 r=[tmpc.b], w=[Osb.b])
                            if lastg:
                                S.op("pool", lambda: pool.tensor_copy(out=yg.t[:, 4 * g:4 * g + 4, h * 64:(h + 1) * 64], in_=Osb.t[:, :, 0:64]),
                                     r=[Osb.b], w=[yg.b])

                        steps.append((S1, S2, S3))
                return steps

            def moba_gates(h, kp, kc):
                for i in range(8, NT):
                    qb = i // 2
                    S.op("pe", lambda: pe.matmul(PS[6].t[:, 0:8], lhsT=qT.t[:, h, i * 128:(i + 1) * 128],
                                                 rhs=ksumb.t[:, kc, :], start=True, stop=True),
                         r=[qT.b, ksumb.b], w=[PS[6].b])
                    S.op("dve", lambda: dve.memset(gate.t[:], -BIG), w=[gate.b])
                    S.op("dve", lambda: dve.tensor_copy(out=gate.t[:, 0:qb], in_=PS[6].t[:, 0:qb]), w=[PS[6].b, gate.b])
                    S.op("dve", lambda: dve.max(out=top8.t[:], in_=gate.t[:]), r=[gate.b], w=[top8.b])
                    S.op("dve", lambda: dve.tensor_scalar(out=Msel.t[:, h, i, :], in0=gate.t[:, 0:8], scalar1=top8.t[:, 2:3], scalar2=None,
                                                          op0=ALU.is_ge),
                         r=[gate.b, top8.b], w=[Msel.b])

            def moba_steps(h, kp, kc):
                Tap = Ttab.t[:, h, :]
                steps = []
                for g in range(4):
                    grp = {"Own": None, "first_own": True, "Pvp": None, "first_pv": True}
                    for b in range(0, 4 * g + 4):
                        st = {}
                        firstg = b == 0
                        lastg = b == 4 * g + 3

                        def F(st=st, b=b, g=g):
                            jlo = max(0, b - 4 * g)
                            ncol = (4 - jlo) * 128
                            q0 = (4 * g + jlo) * 128
                            Sb_ = S.rot("sb3", SB3)
                            S.op("pe", lambda: pe.matmul(Sb_.t[:, 0:ncol], lhsT=kT.t[:, kc, b * 128:(b + 1) * 128],
                                                         rhs=qT.t[:, h, q0:q0 + ncol], start=True, stop=True),
                                 r=[kT.b, qT.b], w=[Sb_.b])
                            e_ = S.rot("eb", Ebufs)
                            S.op("act", lambda: act.activation(out=e_.t[:, 0:ncol], in_=Sb_.t[:, 0:ncol], func=AF.Exp), w=[Sb_.b, e_.b])
                            p_ = S.rot("pb", Pbufs)
                            tau0 = q0 - 128 * b
                            S.op("dve", lambda: dve.tensor_tensor(out=p_.t[:, 0:ncol], in0=e_.t[:, 0:ncol], in1=Tap[:, tau0:tau0 + ncol], op=ALU.mult),
                                 r=[e_.b, Ttab.b], w=[p_.b])
                            st.update(p=p_, jlo=jlo)

                        def B(st=st, b=b, g=g, grp=grp, firstg=firstg, lastg=lastg):
                            p_, jlo = st["p"], st["jlo"]
                            n = b // 2
                            if firstg:
                                grp["Own"] = S.rot("ob2", OB2)
                                S.op("dve", lambda: dve.memset(Osb.t[:], 0.0), w=[Osb.b])
                            if b % 2 == 0:
                                grp["Pvp"] = S.rot("pvb", PVB)
                                grp["first_pv"] = True
                            Own, Pvp = grp["Own"], grp["Pvp"]
                            for j in range(jlo, 4):
                                qb = (4 * g + j) // 2
                                if n == qb or g < 2:
                                    stf = grp["first_own"]
                                    S.op("pe", lambda: pe.matmul(Own.t[:, j * 65:(j + 1) * 65], lhsT=p_.t[:, (j - jlo) * 128:(j - jlo + 1) * 128],
                                                                 rhs=vaug.t[:, b, h, :], start=stf, stop=False, skip_group_check=True),
                                         r=[p_.b, vaug.b], w=[Own.b])
                                    grp["first_own"] = False
                                else:
                                    stf = grp["first_pv"]
                                    S.op("pe", lambda: pe.matmul(Pvp.t[:, j * 65:(j + 1) * 65], lhsT=p_.t[:, (j - jlo) * 128:(j - jlo + 1) * 128],
                                                                 rhs=vaug.t[:, b, h, :], start=stf, stop=False, skip_group_check=True),
                                         r=[p_.b, vaug.b], w=[Pvp.b])
                                    grp["first_pv"] = False
                            if b % 2 == 1 and g >= 2:
                                jA = 0 if n < 2 * g else 2
                                if n < 2 * g + 1:
                                    nj = 4 - jA
                                    Mbc = bass.AP(Msel.t, ((h * NT + 4 * g + jA) * 8 + n), [[4 * NT * 8, 128], [8, nj], [0, 65]])
                                    Pv3 = Pvp.t[:, jA * 65:4 * 65].rearrange("p (j e) -> p j e", e=65)
                                    S.op("dve", lambda: dve.tensor_tensor(out=tmpc.t[:, jA:4, :], in0=Pv3, in1=Mbc, op=ALU.mult),
                                         r=[Msel.b], w=[Pvp.b, tmpc.b])
                                    S.op("dve", lambda: dve.tensor_tensor(out=Osb.t[:, jA:4, :], in0=tmpc.t[:, jA:4, :], in1=Osb.t[:, jA:4, :],
                                                                          op=ALU.add),
                                         r=[tmpc.b], w=[Osb.b])
                            if lastg:
                                finish_group(g, Own, h, add_osb=True)

                        steps.append((F, B))
                return steps

            def group_norm(m):
                pieces = []

                def sq(i):
                    def f():
                        S.op("act", lambda: act.activation(out=junk.t[:, 0:256], in_=yg.t[:, i, :], func=AF.Square, accum_out=ssg.t[:, i:i + 1]),
                             r=[yg.b], w=[junk.b, ssg.b])
                    return f

                def stats():
                    S.op("act", lambda: act.activation(out=rsg.t[:], in_=ssg.t[:], func=AF.Ln, scale=1.0 / 256, bias=epsc.t[:, 0:1]),
                         r=[ssg.b, epsc.b], w=[rsg.b])
                    S.op("act", lambda: act.activation(out=rsg.t[:], in_=rsg.t[:], func=AF.Exp, scale=-0.5), w=[rsg.b])

                def tr(i):
                    def f():
                        xn = S.rot("xn", xns)
                        S.op("dve", lambda: dve.scalar_tensor_tensor(out=xn.t[:, 0:256], in0=yg.t[:, i, :], scalar=rsg.t[:, i:i + 1],
                                                                     in1=gGrp.t[:, m * 256:(m + 1) * 256], op0=ALU.mult, op1=ALU.mult),
                             r=[yg.b, rsg.b, gGrp.b], w=[xn.b])
                        transpose_to(yT.t[:, 2 * m:2 * m + 2, i * 128:(i + 1) * 128], yT.b, xn, 2, eng=("dve" if i % 2 else "act"))
                    return f

                for i in range(0, NT, 4):
                    pieces.append(lambda i=i: [sq(j)() for j in range(i, i + 4)])
                pieces.append(stats)
                for i in range(NT):
                    pieces.append(tr(i))
                return pieces

            def interleave(main, side, every=2):
                si = 0
                for ci, ch in enumerate(main):
                    ch()
                    if ci % every == every - 1 and si < len(side):
                        side[si]()
                        si += 1
                while si < len(side):
                    side[si]()
                    si += 1

            for s in range(0 if _os.environ.get("T_SKIPMIX") else n_seq):
                for i in range(NT):
                    xt = S.rot("xt", xts)
                    S.dma("sp", xt.t[:], x_src.ap()[s, i * 128:(i + 1) * 128, :],
                          r=([x_src_b[s][i]] if x_src_b else none_b), w=[xt.b])
                    k2 = i % 2
                    rms_rstd(xt.t[:], D, junk, ssx.t[:, k2:k2 + 1], ssx.b, rsx.t[:, k2:k2 + 1], rsx.b, xt.b)
                    xn = S.rot("xn", xns)
                    S.op("dve", lambda: dve.scalar_tensor_tensor(out=xn.t[:], in0=xt.t[:], scalar=rsx.t[:, k2:k2 + 1], in1=gMix.t[:],
                                                                 op0=ALU.mult, op1=ALU.mult),
                         r=[xt.b, rsx.b, gMix.b], w=[xn.b])
                    transpose_to(hT.t[:, :, i * 128:(i + 1) * 128], hT.b, xn, 8, eng=("dve" if i % 2 else "act"))

                ei = [0]
                load_wslice(A_Q, 768)
                S.dma("sp", Ttab.t[:], Texp.ap()[0:4].rearrange("h p t -> p h t"), r=Texp_b[0:4], w=[Ttab.b])
                interleave(proj_T(qT, 2, 0, 0.125, ei) + proj_T(kT, 2, 256, 1.0, ei) + proj_V(512, 4, ei), [])
                load_wslice(B_Q, 768)
                steps = []
                for h in range(4):
                    steps += softmax_steps(h, (h % 2) * 64, h // 2, (h % 2) * 64, h, h, Ttab.t[:, h, :], Ttab.b, 15)
                run_pipeline(steps, [2])
                S.dma("sp", Ttab.t[:], Texp.ap()[8:12].rearrange("h p t -> p h t"), r=Texp_b[8:12], w=[Ttab.b])
                gn_pending = group_norm(0)
                if dbg and s == 0 and l == 0:
                    S.dma("sp", dbg_out["yg"].ap()[0].rearrange("(i p) f -> p i f", p=128), yg.t[:], r=[yg.b])
                interleave(proj_T(qT, 2, 0, 0.125, ei) + proj_T(kT, 2, 256, 1.0, ei) + proj_V(512, 4, ei), gn_pending)
                load_wslice(C_Q, 512)
                steps = []
                for h in range(4):
                    steps += stick_steps(h, (h % 2) * 64, h // 2)
                run_pipeline(steps, [1, 1])
                gn_pending = group_norm(1)
                if dbg and s == 0 and l == 0:
                    S.dma("sp", dbg_out["yg"].ap()[1].rearrange("(i p) f -> p i f", p=128), yg.t[:], r=[yg.b])
                interleave(proj_T(qT, 2, 0, 0.125, ei) + proj_T(kT, 1, 256, 1.0, ei) + proj_V(384, 2, ei), gn_pending)
                load_wslice(D_Q, 768)
                steps = []
                for h in range(4):
                    kvh = h // 2
                    qc = {0: 0, 2: 1, 1: 2, 3: 3}[h]
                    steps += softmax_steps(h, kvh * 64, 0, kvh * 64, qc, kvh, TtabC.t[:, h, :], TtabC.b, 1,
                                           extra=expsink.t[:, l * 4 + h:l * 4 + h + 1])
                run_pipeline(steps, [2])
                gn_pending = group_norm(2)
                if dbg and s == 0 and l == 0:
                    S.dma("sp", dbg_out["yg"].ap()[2].rearrange("(i p) f -> p i f", p=128), yg.t[:], r=[yg.b])
                interleave(proj_T(qT, 2, 0, 0.125, ei) + proj_T(kT, 2, 256, 1.0, ei) + proj_V(512, 4, ei), gn_pending)
                S.op("dve", lambda: dve.tensor_reduce(out=ksum.t[:], in_=kT.t[:].rearrange("p c (n k) -> p c n k", k=256),
                                                      axis=mybir.AxisListType.X, op=ALU.add),
                     r=[kT.b], w=[ksum.b])
                S.op("dve", lambda: dve.tensor_copy(out=ksumb.t[:], in_=ksum.t[:]), r=[ksum.b], w=[ksumb.b])
                steps = []
                for h in range(4):
                    moba_gates(h, (h % 2) * 64, h // 2)
                for h in range(4):
                    steps += moba_steps(h, (h % 2) * 64, h // 2)
                run_pipeline(steps, [2])
                interleave([], group_norm(3))
                if dbg and s == 0 and l == 0:
                    S.dma("sp", dbg_out["yg"].ap()[3].rearrange("(i p) f -> p i f", p=128), yg.t[:], r=[yg.b])
                OPB = [PS[4], PS[5], PS[0], PS[1]]
                xt_of = {}

                def load_x(i):
                    xt_ = S.rot("xt", xts)
                    S.dma("sp", xt_.t[:], x_src.ap()[s, i * 128:(i + 1) * 128, :],
                          r=([x_src_b[s][i]] if x_src_b else none_b), w=[xt_.b])
                    xt_of[i] = xt_

                load_x(0)
                for i in range(NT):
                    if i + 1 < NT:
                        load_x(i + 1)
                    xt = xt_of[i]
                    for hf in range(2):
                        pb = S.rot("opb", OPB)
                        for c in range(8):
                            S.op("pe", lambda: pe.matmul(pb.t[:, :], lhsT=yT.t[:, c, i * 128:(i + 1) * 128],
                                                         rhs=wout.t[:, c, hf * 512:(hf + 1) * 512], start=(c == 0), stop=(c == 7)),
                                 r=[yT.b, wout.b], w=[pb.b])
                        S.op("dve", lambda: dve.tensor_tensor(out=xt.t[:, hf * 512:(hf + 1) * 512], in0=pb.t[:, :],
                                                              in1=xt.t[:, hf * 512:(hf + 1) * 512], op=ALU.add),
                             w=[pb.b, xt.b])
                    S.dma("sp", xA.ap()[s, i * 128:(i + 1) * 128, :], xt.t[:], r=[xt.b], w=[xA_b[s][i]])
                    if dbg and l == 0:
                        S.dma("sp", dbg_out["x1"].ap()[s, i * 128:(i + 1) * 128, :], xt.t[:], r=[xt.b])
            S.barrier()

        with contextlib.ExitStack() as ks:
            gMem = sb(ks, "gMem", [128, D], F32)
            wxkv = sb(ks, "wxkv", [128, 8, 512], BF16)
            mT = sb(ks, "mT", [128, 8, MEM], BF16)
            xts = sbn(ks, "mxt", [128, D], F32, 2)
            xns = sbn(ks, "mxn", [128, D], BF16, 2)
            junk = sb(ks, "mjunk", [128, D], BF16)
            ssx = sb(ks, "mssx", [128, 2], F32)
            rsx = sb(ks, "mrsx", [128, 2], F32)
            load_gain(gMem, gvec["g_mem"], l)
            S.dma("pool", wxkv.t[:], w_view(w_xkv_d, l, 0, D), w=[wxkv.b])
            S.op("dve", lambda: dve.memset(vxa.t[:], 1.0), w=[vxa.b])
            for s in range(n_seq):
                for i in range(2):
                    xt = S.rot("mxt", xts)
                    S.dma("sp", xt.t[:], mem_in.ap()[s, i * 128:(i + 1) * 128, :], w=[xt.b])
                    rms_rstd(xt.t[:], D, junk, ssx.t[:, i:i + 1], ssx.b, rsx.t[:, i:i + 1], rsx.b, xt.b)
                    xn = S.rot("mxn", xns)
                    S.op("dve", lambda: dve.scalar_tensor_tensor(out=xn.t[:], in0=xt.t[:], scalar=rsx.t[:, i:i + 1], in1=gMem.t[:],
                                                                 op0=ALU.mult, op1=ALU.mult),
                         r=[xt.b, rsx.b, gMem.b], w=[xn.b])
                    transpose_to(mT.t[:, :, i * 128:(i + 1) * 128], mT.b, xn, 8)
                for cc in range(2):
                    pb = S.rot("psm", PS_M)
                    for c in range(8):
                        S.op("pe", lambda: pe.matmul(pb.t[:, 0:MEM], lhsT=wxkv.t[:, c, cc * 128:(cc + 1) * 128], rhs=mT.t[:, c, :],
                                                     start=(c == 0), stop=(c == 7)),
                             r=[wxkv.b, mT.b], w=[pb.b])
                    S.op("dve", lambda: dve.tensor_copy(out=kxT.t[:, s, cc, :], in_=pb.t[:, 0:MEM]), w=[pb.b, kxT.b])
                for i in range(2):
                    pb = S.rot("psm", PS_M)
                    for c in range(8):
                        S.op("pe", lambda: pe.matmul(pb.t[:, 0:256], lhsT=mT.t[:, c, i * 128:(i + 1) * 128], rhs=wxkv.t[:, c, 256:512],
                                                     start=(c == 0), stop=(c == 7)),
                             r=[wxkv.b, mT.b], w=[pb.b])
                    S.op("dve", lambda: dve.tensor_copy(out=vxa.t[:, s, i, :, 0:64],
                                                        in_=pb.t[:, 0:256].rearrange("p (h e) -> p h e", e=64)),
                         w=[pb.b, vxa.b])
            S.barrier()

        with contextlib.ExitStack() as ts:
            gFin = sb(ts, "gFin", [128, D], F32) if last else None
            gCT = sb(ts, "gCT", [128, 8], F32)
            gMT = sb(ts, "gMT", [128, 8], F32)
            wup = sb(ts, "wup", [128, 8, DFF], BF16)
            wdn = sb(ts, "wdn", [128, 32, D], BF16)
            wxq = sb(ts, "wxq", [128, 8, 256], BF16)
            wxo = sb(ts, "wxo", [128, 2, D], BF16)
            xgs = sbn(ts, "xg", [128, 2, D], F32, 2)
            xnk = sbn(ts, "txn", [128, D], BF16, 2)
            hTc = sb(ts, "hTc", [128, 8, 256], BF16)
            hTms = sbn(ts, "hTm", [128, 8, 256], BF16, 2)
            qxT = sb(ts, "qxT", [128, 4, 256], BF16)
            Eb = sbn(ts, "tEb", [128, 512], BF16, 2)
            oxn = sb(ts, "oxn", [128, 256], BF16)
            oxT = sb(ts, "oxT", [128, 2, 256], BF16)
            aT = sb(ts, "aT", [128, 32, 256], BF16)
            ssx = sb(ts, "tssx", [128, 2], F32)
            ssf = sb(ts, "tssf", [128, 2], F32)
            rsf = sb(ts, "trsf", [128, 2], F32)
            fjunk = sb(ts, "fjunk", [128, D], BF16)
            rsx = sb(ts, "trsx", [128, 2], F32)
            dden = sb(ts, "tdden", [128, 4], F32)
            rl = sbn(ts, "rl", [128, 256], F32, 2)
            S.op("pool", lambda: pool.memset(qxT.t[:], 0.0), w=[qxT.b])
            S.dma("sp", gCT.t[:], gT_in["gT_cross"].ap()[l], w=[gCT.b])
            S.dma("sp", gMT.t[:], gT_in["gT_mlp"].ap()[l], w=[gMT.b])
            if last:
                load_gain(gFin, gfin_in, 0)
            S.dma("pool", wxq.t[:], w_view(w_xq_d, l, 0, D), w=[wxq.b])
            S.dma("pool", wxo.t[:], w_view(w_xo_d, l, 0, 256), w=[wxo.b])
            for c4 in range(4):
                S.dma("pool", wup.t[:, :, c4 * 1024:(c4 + 1) * 1024],
                      w_up_d.ap()[l, :, c4 * 1024:(c4 + 1) * 1024].rearrange("(c p) n -> p c n", p=128), w=[wup.b])
            for c4 in range(4):
                S.dma("pool", wdn.t[:, c4 * 8:(c4 + 1) * 8, :], w_view(w_dn_d, l, c4 * 1024, 1024), w=[wdn.b])

            FS = [PS[0], PS[1]]
            FO = PS[2]
            UPB = [PS[4], PS[5]]
            DNB = [PS[3], PS[7]]
            groups = [(s, tg) for s in range(n_seq) for tg in range(SEQ // 256)]

            def norm_stage(xg, k, xn):
                def f():
                    S.op("act", lambda: act.activation(out=xn.t[:], in_=xg.t[:, k, :], func=AF.Square, accum_out=ssx.t[:, k:k + 1]),
                         r=[xg.b], w=[xn.b, ssx.b])
                    S.op("act", lambda: act.activation(out=rsx.t[:, k:k + 1], in_=ssx.t[:, k:k + 1], func=AF.Ln, scale=1.0 / D,
                                                       bias=epsc.t[:, 0:1]), r=[ssx.b, epsc.b], w=[rsx.b])
                    S.op("act", lambda: act.activation(out=rsx.t[:, k:k + 1], in_=rsx.t[:, k:k + 1], func=AF.Exp, scale=-0.5),
                         w=[rsx.b])
                    S.op("dve", lambda: dve.tensor_scalar(out=xn.t[:], in0=xg.t[:, k, :], scalar1=rsx.t[:, k:k + 1], scalar2=None,
                                                          op0=ALU.mult), r=[xg.b, rsx.b], w=[xn.b])
                return f

            def transp_stage(xn, dst, k, gT):
                def f():
                    pb = PS[6].t.bitcast(BF16)
                    for c in range(8):
                        S.op("pe", lambda: pe.transpose(out=pb[:, c * 128:(c + 1) * 128], in_=xn.t[:, c * 128:(c + 1) * 128],
                                                        identity=ident.t[:]), r=[xn.b, ident.b], w=[PS[6].b])
                    for c in range(8):
                        if c % 2 == 0 or _os.environ.get('T_NOACT'):
                            S.op("dve", lambda: dve.tensor_scalar(out=dst.t[:, c, k * 128:(k + 1) * 128], in0=pb[:, c * 128:(c + 1) * 128],
                                                                  scalar1=gT.t[:, c:c + 1], scalar2=None, op0=ALU.mult),
                                 r=[gT.b], w=[PS[6].b, dst.b])
                        else:
                            S.op("act", lambda: act.activation(out=dst.t[:, c, k * 128:(k + 1) * 128], in_=pb[:, c * 128:(c + 1) * 128],
                                                               func=AF.Identity, scale=gT.t[:, c:c + 1]),
                                 r=[gT.b], w=[PS[6].b, dst.b])
                return f

            def make_F(t):
                s, tg = groups[t]
                xg = xgs[t % 2]
                hTm = hTms[t % 2]
                st = []

                def load(k):
                    def f():
                        i = tg * 2 + k
                        S.dma("sp", xg.t[:, k, :], xA.ap()[s, i * 128:(i + 1) * 128, :], r=[xA_b[s][i]], w=[xg.b])
                    return f

                def qproj():
                    for cc in range(2):
                        for c in range(8):
                            S.op("pe", lambda: pe.matmul(FO.t[:, cc * 256:(cc + 1) * 256], lhsT=wxq.t[:, c, cc * 128:(cc + 1) * 128],
                                                         rhs=hTc.t[:, c, :], start=(c == 0 and cc == 0), stop=(c == 7),
                                                         skip_group_check=True),
                                 r=[wxq.b, hTc.b], w=[FO.b])
                    FOv = FO.t[:, :].rearrange("p (c t) -> p c t", t=256)
                    S.op("dve", lambda: dve.tensor_scalar(out=qxT.t[0:64, 0:4:2, :], in0=FOv[0:64, :, :],
                                                          scalar1=0.125, scalar2=None, op0=ALU.mult), w=[FO.b, qxT.b])
                    S.op("dve", lambda: dve.tensor_scalar(out=qxT.t[64:128, 1:4:2, :], in0=FOv[64:128, :, :],
                                                          scalar1=0.125, scalar2=None, op0=ALU.mult), w=[FO.b, qxT.b])

                def scores(k):
                    def f():
                        for idx in range(8):
                            h, mt = idx // 2, idx % 2
                            hp, hc = (h % 2) * 64, h // 2
                            bank = FS[h % 2]
                            col = ((h // 2) * 2 + mt) * 128
                            S.op("pe", lambda: pe.matmul(bank.t[:, col:col + 128], lhsT=kxT.t[:, s, hc, mt * 128:(mt + 1) * 128],
                                                         rhs=qxT.t[:, h, k * 128:(k + 1) * 128], start=True, stop=True,
                                                         skip_group_check=True),
                                 r=[kxT.b, qxT.b], w=[bank.b])
                        for bi in range(2):
                            S.op("act", lambda: act.activation(out=Eb[bi].t[:, :], in_=FS[bi].t[:, :], func=AF.Exp), w=[FS[bi].b, Eb[bi].b])
                    return f

                def pv(k):
                    def f():
                        first = True
                        for idx in range(8):
                            h, mt = idx // 2, idx % 2
                            e_ = Eb[h % 2]
                            col = ((h // 2) * 2 + mt) * 128
                            stf = first
                            S.op("pe", lambda: pe.matmul(FO.t[:, h * 65:(h + 1) * 65], lhsT=e_.t[:, col:col + 128], rhs=vxa.t[:, s, mt, h, :],
                                                         start=stf, stop=False, skip_group_check=True),
                                 r=[e_.b, vxa.b], w=[FO.b])
                            first = False
                        Ov = FO.t[:, 0:260].rearrange("p (j e) -> p j e", e=65)
                        S.op("dve", lambda: dve.reciprocal(out=dden.t[:], in_=Ov[:, :, 64]), w=[FO.b, dden.b])
                        for h in range(4):
                            S.op("dve", lambda: dve.tensor_scalar(out=oxn.t[:, h * 64:(h + 1) * 64], in0=Ov[:, h, 0:64],
                                                                  scalar1=dden.t[:, h:h + 1], scalar2=None, op0=ALU.mult),
                                 r=[dden.b], w=[FO.b, oxn.b])
                        pb = PS[6].t.bitcast(BF16)
                        for c in range(2):
                            S.op("pe", lambda: pe.transpose(out=pb[:, c * 128:(c + 1) * 128], in_=oxn.t[:, c * 128:(c + 1) * 128],
                                                            identity=ident.t[:]), r=[oxn.b, ident.b], w=[PS[6].b])
                        S.op("dve", lambda: dve.tensor_copy(out=oxT.t[:, :, k * 128:(k + 1) * 128],
                                                            in_=pb[:, 0:256].rearrange("p (c t) -> p c t", t=128)),
                             w=[PS[6].b, oxT.b])
                    return f

                def oproj(k):
                    def f():
                        for hf in range(2):
                            for c in range(2):
                                S.op("pe", lambda: pe.matmul(FO.t[:, :], lhsT=oxT.t[:, c, k * 128:(k + 1) * 128],
                                                             rhs=wxo.t[:, c, hf * 512:(hf + 1) * 512], start=(c == 0), stop=(c == 1)),
                                     r=[oxT.b, wxo.b], w=[FO.b])
                            S.op("dve", lambda: dve.tensor_tensor(out=xg.t[:, k, hf * 512:(hf + 1) * 512], in0=FO.t[:, :],
                                                                  in1=xg.t[:, k, hf * 512:(hf + 1) * 512], op=ALU.add),
                                 w=[FO.b, xg.b])
                    return f

                st.append(load(0))
                st.append(load(1))
                st.append(norm_stage(xg, 0, xnk[0]))
                st.append(norm_stage(xg, 1, xnk[1]))
                st.append(transp_stage(xnk[0], hTc, 0, gCT))
                st.append(transp_stage(xnk[1], hTc, 1, gCT))
                st.append(qproj)
                st.append(scores(0))
                st.append(pv(0))
                st.append(scores(1))
                st.append(pv(1))
                st.append(oproj(0))
                st.append(oproj(1))
                st.append(norm_stage(xg, 0, xnk[0]))
                st.append(norm_stage(xg, 1, xnk[1]))
                st.append(transp_stage(xnk[0], hTm, 0, gMT))
                st.append(transp_stage(xnk[1], hTm, 1, gMT))
                return st

            def make_M(t):
                s, tg = groups[t]
                xg = xgs[t % 2]
                hTm = hTms[t % 2]
                ch = []

                def up(fc):
                    def f():
                        pb = S.rot("upb", UPB)
                        for c in range(8):
                            S.op("pe", lambda: pe.matmul(pb.t[:, 0:256], lhsT=wup.t[:, c, fc * 128:(fc + 1) * 128], rhs=hTm.t[:, c, :],
                                                         start=(c == 0), stop=(c == 7)),
                                 r=[wup.b, hTm.b], w=[pb.b])
                        r_ = S.rot("rl", rl)
                        if fc % 2 == 0:
                            S.op("act", lambda: act.activation(out=r_.t[:, :], in_=pb.t[:, 0:256], func=AF.Relu), w=[pb.b, r_.b])
                            S.op("pool", lambda: pool.tensor_tensor(out=aT.t[:, fc, :], in0=r_.t[:, :], in1=r_.t[:, :], op=ALU.mult),
                                 r=[r_.b], w=[aT.b])
                        else:
                            S.op("dve", lambda: dve.tensor_scalar(out=r_.t[:, :], in0=pb.t[:, 0:256], scalar1=0.0, scalar2=None, op0=ALU.max),
                                 w=[pb.b, r_.b])
                            S.op("dve", lambda: dve.tensor_tensor(out=aT.t[:, fc, :], in0=r_.t[:, :], in1=r_.t[:, :], op=ALU.mult),
                                 r=[r_.b], w=[aT.b])
                    return f

                def down(k, hf):
                    def f():
                        pb = S.rot("dnb", DNB)
                        for fc in range(32):
                            S.op("pe", lambda: pe.matmul(pb.t[:, :], lhsT=aT.t[:, fc, k * 128:(k + 1) * 128],
                                                         rhs=wdn.t[:, fc, hf * 512:(hf + 1) * 512], start=(fc == 0), stop=(fc == 31)),
                                 r=[aT.b, wdn.b], w=[pb.b])
                        S.op("dve", lambda: dve.tensor_tensor(out=xg.t[:, k, hf * 512:(hf + 1) * 512], in0=pb.t[:, :],
                                                              in1=xg.t[:, k, hf * 512:(hf + 1) * 512], op=ALU.add),
                             w=[pb.b, xg.b])
                        if hf == 1:
                            i = tg * 2 + k
                            if last:
                                S.op("act", lambda: act.activation(out=fjunk.t[:], in_=xg.t[:, k, :], func=AF.Square, accum_out=ssf.t[:, k:k + 1]),
                                     r=[xg.b], w=[fjunk.b, ssf.b])
                                S.op("act", lambda: act.activation(out=rsf.t[:, k:k + 1], in_=ssf.t[:, k:k + 1], func=AF.Ln, scale=1.0 / D,
                                                                   bias=epsc.t[:, 0:1]), r=[ssf.b, epsc.b], w=[rsf.b])
                                S.op("act", lambda: act.activation(out=rsf.t[:, k:k + 1], in_=rsf.t[:, k:k + 1], func=AF.Exp, scale=-0.5),
                                     w=[rsf.b])
                                S.op("dve", lambda: dve.scalar_tensor_tensor(out=xg.t[:, k, :], in0=xg.t[:, k, :], scalar=rsf.t[:, k:k + 1],
                                                                             in1=gFin.t[:], op0=ALU.mult, op1=ALU.mult),
                                     r=[rsf.b, gFin.b], w=[xg.b])
                                S.dma("sp", y_out.ap()[s, i * 128:(i + 1) * 128, :], xg.t[:, k, :], r=[xg.b], w=[y_b[s][i]])
                            else:
                                S.dma("sp", xB.ap()[s, i * 128:(i + 1) * 128, :], xg.t[:, k, :], r=[xg.b], w=[xB_b[s][i]])
                    return f

                for fc in range(32):
                    ch.append(up(fc))
                for k in range(2):
                    for hf in range(2):
                        ch.append(down(k, hf))
                return ch

            _stop = int(_os.environ.get("T_STOP", "999"))
            for f in make_F(0)[:_stop]:
                f()
            for t in range(len(groups) if _stop >= 999 else 0):
                Mt = make_M(t)
                Fn = make_F(t + 1) if t + 1 < len(groups) else []
                fi = 0
                for ci, chunk in enumerate(Mt):
                    chunk()
                    if ci % 2 == 1 and fi < len(Fn) and not _os.environ.get('T_NOINTER'):
                        Fn[fi]()
                        fi += 1
                while fi < len(Fn):
                    Fn[fi]()
                    fi += 1
            S.barrier()
    S.barrier()
    return nc


_C_PERM = np.concatenate([np.arange(0, 64), np.arange(128, 192), np.arange(64, 128), np.arange(192, 256)])


def make_in_maps(inputs, n_cores, n_seq):
    f32 = np.float32
    w_in = np.asarray(inputs["w_in"], f32)
    perm = np.arange(IN_W)
    perm[C_Q:C_Q + 256] = C_Q + _C_PERM
    shared = {
        "rel_table": np.ascontiguousarray(inputs["rel_table"], f32),
        "g_final": np.asarray(inputs["g_final"], f32).reshape(1, D),
        "sinks": np.asarray(inputs["sinks"], f32).reshape(1, DEPTH * 4),
        "w_in": np.ascontiguousarray(w_in[:, :, perm]),
    }
    for k in ("g_mix", "g_group", "g_cross", "g_mem", "g_mlp", "w_out", "w_xq", "w_xkv", "w_xo", "w_up", "w_down"):
        shared[k] = np.ascontiguousarray(inputs[k], f32)
    for k, src in (("gT_cross", "g_cross"), ("gT_mlp", "g_mlp")):
        shared[k] = np.ascontiguousarray(np.asarray(inputs[src], f32).reshape(DEPTH, 8, 128).transpose(0, 2, 1))
    shared.update(_host_consts())
    x = np.asarray(inputs["x"], f32)
    mem = np.asarray(inputs["mem"], f32)
    maps = []
    for c in range(n_cores):
        m = dict(shared)
        m["x"] = np.ascontiguousarray(x[c * n_seq:(c + 1) * n_seq])
        m["mem"] = np.ascontiguousarray(mem[c * n_seq:(c + 1) * n_seq])
        maps.append(m)
    return maps


def kernel(**inputs):
    B = inputs["x"].shape[0]
    n_seq = B // N_CORES
    nc = build_program(n_seq)
    maps = make_in_maps(inputs, N_CORES, n_seq)
    res = run_bass_kernel_spmd(nc, maps, core_ids=list(range(N_CORES)))
    return np.concatenate([np.asarray(r["y"], np.float32) for r in res.results], axis=0)
```

```python
import math
import os as _os
import contextlib
import numpy as np
import ml_dtypes
import concourse.bass as bass
import concourse.mybir as mybir
from concourse.bass_utils import run_bass_kernel_spmd

F32 = mybir.dt.float32
BF16 = mybir.dt.bfloat16
AF = mybir.ActivationFunctionType
ALU = mybir.AluOpType

D = 1024
SEQ = 2048
NT = SEQ // 128
MEM = 256
DEPTH = 2
IN_W = 2816
DFF = 4096
NDIST = SEQ + 127
EPS = 1e-6
N_CORES = 8
BIG = 1.0e30

A_Q, A_K, A_V = 0, 256, 512
B_Q, B_K, B_V = 768, 1024, 1280
C_Q, C_K, C_V = 1536, 1792, 1920
D_Q, D_K, D_V = 2048, 2304, 2560


class Buf:
    __slots__ = ("name", "lw", "rd")

    def __init__(self, name):
        self.name = name
        self.lw = None
        self.rd = {}


class Tile:
    def __init__(self, t, b):
        self.t = t
        self.b = b


class Sched:
    EPOCH = 16000

    def __init__(self, nc, es):
        self.nc = nc
        self.es = es
        self.eng = {"pe": nc.tensor, "act": nc.scalar, "dve": nc.vector, "pool": nc.gpsimd, "sp": nc.sync}
        self.cnt = {k: 0 for k in self.eng}
        self.sems = {k: [] for k in self.eng}
        self.seen = {k: {} for k in self.eng}
        self.seen_dma = {k: set() for k in self.eng}
        self.q = {}
        self.rr = {}

    def newsem(self, name):
        return self.es.enter_context(self.nc.semaphore(name))

    def add_queue(self, name, issuer, K=8):
        self.q[name] = dict(issuer=issuer, sems=[self.newsem(f"dq_{name}_{i}") for i in range(K)], n=0, K=K)

    def _sem_for(self, e, c):
        idx = (c - 1) // self.EPOCH
        while len(self.sems[e]) <= idx:
            self.sems[e].append(self.newsem(f"s_{e}_{len(self.sems[e])}"))
        return self.sems[e][idx], c - idx * self.EPOCH

    def _wait(self, e, tok):
        if tok[0] == "e":
            _, x, c = tok
            if e == "pe" and x == "pe":
                return
            if self.seen[e].get(x, 0) >= c:
                return
            sem, v = self._sem_for(x, c)
            self.eng[e].wait_ge(sem, v)
            self.seen[e][x] = c
        else:
            _, qn, i = tok
            if tok in self.seen_dma[e]:
                return
            Q = self.q[qn]
            self.eng[e].wait_ge(Q["sems"][i % Q["K"]], 16 * (i // Q["K"] + 1))
            self.seen_dma[e].add(tok)

    @staticmethod
    def _deps(r, w):
        toks = []
        for b in r:
            if b.lw is not None:
                toks.append(b.lw)
        for b in w:
            if b.lw is not None:
                toks.append(b.lw)
            toks.extend(b.rd.values())
        return toks

    @staticmethod
    def _commit(tok, key, r, w):
        for b in w:
            b.lw = tok
            b.rd = {}
        for b in r:
            if b not in w:
                b.rd[key] = tok

    def op(self, e, fn, r=(), w=()):
        for t in self._deps(r, w):
            self._wait(e, t)
        inst = fn()
        self.cnt[e] += 1
        c = self.cnt[e]
        sem, _ = self._sem_for(e, c)
        inst.then_inc(sem, 1)
        self._commit(("e", e, c), e, r, w)

    def dma(self, qn, out, in_, r=(), w=()):
        Q = self.q[qn]
        e = Q["issuer"]
        i = Q["n"]
        if i >= Q["K"]:
            self._wait(e, ("d", qn, i - Q["K"]))
        for t in self._deps(r, w):
            self._wait(e, t)
        inst = self.eng[e].dma_start(out=out, in_=in_)
        inst.then_inc(Q["sems"][i % Q["K"]], 16)
        Q["n"] += 1
        tok = ("d", qn, i)
        self._commit(tok, tok, r, w)

    def barrier(self):
        for e in self.eng:
            for x in self.eng:
                if x != e and self.cnt[x] > 0:
                    self._wait(e, ("e", x, self.cnt[x]))
            for qn, Q in self.q.items():
                for i in range(max(0, Q["n"] - Q["K"]), Q["n"]):
                    self._wait(e, ("d", qn, i))

    def rot(self, key, lst):
        i = self.rr.get(key, 0)
        self.rr[key] = i + 1
        return lst[i % len(lst)]


def _rel_bucket_np(dist):
    max_exact = 16
    n = np.maximum(dist, 0)
    nf = np.maximum(n, 1).astype(np.float32)
    large = max_exact + (np.log(nf / np.float32(max_exact)) / np.float32(math.log(2048 / max_exact))
                         * np.float32(32 - max_exact)).astype(np.int32)
    large = np.minimum(large, 31)
    return np.where(n < max_exact, n, large)


def _host_consts():
    bf = ml_dtypes.bfloat16
    idx = np.arange(128)
    ident = np.eye(128, dtype=np.float32)
    jrev = ident[::-1].copy()
    trineg = -(idx[:, None] >= idx[None, :]).astype(np.float32)
    maskb = (idx[:, None] < idx[None, :]).astype(np.float32)
    dist = np.arange(NDIST) - 127
    bucket = _rel_bucket_np(dist)
    oh = np.zeros((32, NDIST), np.float32)
    oh[bucket, np.arange(NDIST)] = 1.0
    mult = np.zeros((12, NDIST), np.float32)
    ge0 = dist >= 0
    ma = ((dist <= 128) & ge0).astype(np.float32) + ((dist % 4 == 0) & (dist <= 512) & ge0) + ((dist % 16 == 0) & ge0)
    mult[0:4] = ma
    mult[4:8] = (ge0 & (dist <= 127)).astype(np.float32)
    mult[8:12] = ge0.astype(np.float32)
    return {
        "c_ident": ident.astype(bf),
        "c_jrev": jrev,
        "c_trineg": trineg.astype(bf),
        "c_maskb": maskb.astype(bf),
        "c_oh": oh,
        "c_mult": mult,
    }


def build_program(n_seq, n_layers=DEPTH, dbg=False):
    nc = bass.Bass("TRN2", target_bir_lowering=False)
    es = contextlib.ExitStack()
    S = Sched(nc, es)
    S.add_queue("sp", "sp", K=8)
    S.add_queue("pool", "pool", K=4)

    def dram(name, shape, dt, kind):
        return nc.dram_tensor(name, list(shape), dt, kind=kind)

    x_in = dram("x", [n_seq, SEQ, D], F32, "ExternalInput")
    mem_in = dram("mem", [n_seq, MEM, D], F32, "ExternalInput")
    rel_in = dram("rel_table", [32, 12], F32, "ExternalInput")
    gvec = {k: dram(k, [DEPTH, D], F32, "ExternalInput") for k in ("g_mix", "g_group", "g_cross", "g_mem", "g_mlp")}
    gfin_in = dram("g_final", [1, D], F32, "ExternalInput")
    gT_in = {k: dram(k, [DEPTH, 128, 8], F32, "ExternalInput") for k in ("gT_cross", "gT_mlp")}
    sinks_in = dram("sinks", [1, DEPTH * 4], F32, "ExternalInput")
    w_in_d = dram("w_in", [DEPTH, D, IN_W], F32, "ExternalInput")
    w_out_d = dram("w_out", [DEPTH, D, D], F32, "ExternalInput")
    w_xq_d = dram("w_xq", [DEPTH, D, 256], F32, "ExternalInput")
    w_xkv_d = dram("w_xkv", [DEPTH, D, 512], F32, "ExternalInput")
    w_xo_d = dram("w_xo", [DEPTH, 256, D], F32, "ExternalInput")
    w_up_d = dram("w_up", [DEPTH, D, DFF], F32, "ExternalInput")
    w_dn_d = dram("w_down", [DEPTH, DFF, D], F32, "ExternalInput")
    c_ident = dram("c_ident", [128, 128], BF16, "ExternalInput")
    c_jrev = dram("c_jrev", [128, 128], F32, "ExternalInput")
    c_trineg = dram("c_trineg", [128, 128], BF16, "ExternalInput")
    c_maskb = dram("c_maskb", [128, 128], BF16, "ExternalInput")
    c_oh = dram("c_oh", [32, NDIST], F32, "ExternalInput")
    c_mult = dram("c_mult", [12, NDIST], F32, "ExternalInput")
    y_out = dram("y", [n_seq, SEQ, D], F32, "ExternalOutput")
    xA = dram("xA", [n_seq, SEQ, D], F32, "Internal")
    xB = dram("xB", [n_seq, SEQ, D], F32, "Internal")
    Fd = dram("Fd", [12, NDIST + 1], F32, "Internal")
    Texp = dram("Texp", [12, 128, SEQ], BF16, "Internal")
    dbg_out = {}
    if dbg:
        dbg_out["x1"] = dram("dbg_x1", [n_seq, SEQ, D], F32, "ExternalOutput")
        dbg_out["yg"] = dram("dbg_yg", [4, SEQ, 256], F32, "ExternalOutput")

    def dbuf(name):
        return [[Buf(f"{name}_{s}_{i}") for i in range(NT)] for s in range(n_seq)]

    xA_b, xB_b, y_b = dbuf("xA"), dbuf("xB"), dbuf("y")
    Texp_b = [Buf(f"Texp{h}") for h in range(12)]
    none_b = []

    uid = [0]

    def sb(stack, name, shape, dt):
        uid[0] += 1
        name = f"{name}_u{uid[0]}"
        t = stack.enter_context(nc.sbuf_tensor(name, list(shape), dt))
        return Tile(t, Buf(name))

    def sbn(stack, name, shape, dt, n):
        return [sb(stack, f"{name}{i}", shape, dt) for i in range(n)]

    PS = []
    for i in range(8):
        t = es.enter_context(nc.psum_tensor(f"ps{i}", [128, 512], F32))
        PS.append(Tile(t, Buf(f"ps{i}")))
    PS_S = [PS[0], PS[1]]
    PS_O = [PS[2], PS[3]]
    PS_M = [PS[4], PS[5]]
    PS_T = PS[6]
    PS_X = PS[7]

    ident = sb(es, "ident", [128, 128], BF16)
    trineg = sb(es, "trineg", [128, 128], BF16)
    maskb = sb(es, "maskb", [128, 128], BF16)
    onescol = sb(es, "onescol", [128, 2], BF16)
    expsink = sb(es, "expsink", [128, DEPTH * 4], F32)
    epsc = sb(es, "epsc", [128, 1], F32)

    act = nc.scalar
    dve = nc.vector
    pool = nc.gpsimd
    pe = nc.tensor

    with contextlib.ExitStack() as ps_:
        S.dma("sp", ident.t[:], c_ident.ap(), w=[ident.b])
        S.dma("sp", trineg.t[:], c_trineg.ap(), w=[trineg.b])
        S.dma("sp", maskb.t[:], c_maskb.ap(), w=[maskb.b])
        S.op("dve", lambda: dve.memset(onescol.t[:], 1.0), w=[onescol.b])
        S.op("dve", lambda: dve.memset(epsc.t[:], EPS), w=[epsc.b])
        S.dma("sp", expsink.t[:], sinks_in.ap()[0:1, :].partition_broadcast(128), w=[expsink.b])
        S.op("act", lambda: act.activation(out=expsink.t[:], in_=expsink.t[:], func=AF.Exp), r=[expsink.b], w=[expsink.b])

        tab = sb(ps_, "tab", [32, 12], F32)
        oh = sb(ps_, "oh", [32, NDIST], F32)
        mu = sb(ps_, "mu", [12, NDIST], F32)
        Fs = sb(ps_, "Fs", [12, NDIST + 1], F32)
        jrev = sb(ps_, "jrev", [128, 128], F32)
        Xs = sbn(ps_, "Xs", [128, 512], F32, 2)
        Tb = sbn(ps_, "Tb", [128, 512], BF16, 2)
        S.dma("sp", tab.t[:], rel_in.ap(), w=[tab.b])
        S.dma("sp", oh.t[:], c_oh.ap(), w=[oh.b])
        S.dma("sp", mu.t[:], c_mult.ap(), w=[mu.b])
        S.dma("sp", jrev.t[:], c_jrev.ap(), w=[jrev.b])
        S.op("dve", lambda: dve.memset(Fs.t[:], 0.0), w=[Fs.b])
        for ch in range((NDIST + 511) // 512):
            c0 = ch * 512
            n = min(512, NDIST - c0)
            pb = S.rot("psm", PS_M)
            S.op("pe", lambda: pe.matmul(pb.t[0:12, 0:n], lhsT=tab.t[:, :], rhs=oh.t[:, c0:c0 + n], start=True, stop=True),
                 r=[tab.b, oh.b], w=[pb.b])
            S.op("act", lambda: act.activation(out=Fs.t[:, c0:c0 + n], in_=pb.t[0:12, 0:n], func=AF.Exp), w=[pb.b, Fs.b])
            S.op("dve", lambda: dve.tensor_tensor(out=Fs.t[:, c0:c0 + n], in0=Fs.t[:, c0:c0 + n], in1=mu.t[:, c0:c0 + n], op=ALU.mult),
                 r=[mu.b], w=[Fs.b])
        Fd_b = Buf("Fd")
        S.dma("sp", Fd.ap(), Fs.t[:], r=[Fs.b], w=[Fd_b])
        for rh in range(12):
            W = 256 if 4 <= rh < 8 else SEQ
            for c0 in range(0, W, 512):
                n = min(512, W - c0)
                X = S.rot("Xs", Xs)
                T_ = S.rot("Tb", Tb)
                src = bass.AP(Fd, rh * (NDIST + 1) + c0, [[1, 128], [1, n]])
                S.dma("sp", X.t[:, 0:n], src, r=[Fd_b], w=[X.b])
                pb = S.rot("psm", PS_M)
                S.op("pe", lambda: pe.matmul(pb.t[:, 0:n], lhsT=jrev.t[:], rhs=X.t[:, 0:n], start=True, stop=True),
                     r=[jrev.b, X.b], w=[pb.b])
                S.op("dve", lambda: dve.tensor_copy(out=T_.t[:, 0:n], in_=pb.t[:, 0:n]), w=[pb.b, T_.b])
                S.dma("sp", Texp.ap()[rh, :, c0:c0 + n], T_.t[:, 0:n], r=[T_.b], w=[Texp_b[rh]])
        S.barrier()

    def load_gain(tile_, src_handle, row):
        S.dma("sp", tile_.t[:], src_handle.ap()[row:row + 1, :].partition_broadcast(128), w=[tile_.b])

    def w_view(handle, l, rows0, nrows):
        return handle.ap()[l, rows0:rows0 + nrows, :].rearrange("(c p) n -> p c n", p=128)

    def rms_rstd(x_ap, ncols, junk, ss_t, ss_b, rstd_t, rstd_b, xb):
        S.op("act", lambda: act.activation(out=junk.t[:, 0:ncols], in_=x_ap, func=AF.Square, accum_out=ss_t),
             r=[xb], w=[junk.b, ss_b])
        S.op("act", lambda: act.activation(out=rstd_t, in_=ss_t, func=AF.Ln, scale=1.0 / ncols, bias=epsc.t[:, 0:1]),
             r=[ss_b, epsc.b], w=[rstd_b])
        S.op("act", lambda: act.activation(out=rstd_t, in_=rstd_t, func=AF.Exp, scale=-0.5), r=[rstd_b], w=[rstd_b])

    def transpose_to(dst_ap3, dst_b, src_tile, nchunks, eng="dve"):
        pb = PS_T.t.bitcast(BF16)
        for c in range(nchunks):
            S.op("pe", lambda: pe.transpose(out=pb[:, c * 128:(c + 1) * 128], in_=src_tile.t[:, c * 128:(c + 1) * 128],
                                            identity=ident.t[:]),
                 r=[src_tile.b, ident.b], w=[PS_T.b])
        srcv = pb[:, 0:nchunks * 128].rearrange("p (k t) -> p k t", t=128)
        if eng == "dve":
            S.op("dve", lambda: dve.tensor_copy(out=dst_ap3, in_=srcv), w=[PS_T.b, dst_b])
        else:
            S.op("act", lambda: act.copy(out=dst_ap3, in_=srcv), w=[PS_T.b, dst_b])

    kxT = sb(es, "kxT", [128, n_seq, 2, MEM], BF16)
    vxa = sb(es, "vxa", [128, n_seq, 2, 4, 65], BF16)
    for l in range(n_layers):
        x_src, x_src_b = (x_in, None) if l == 0 else (xB, xB_b)
        last = l == n_layers - 1

        with contextlib.ExitStack() as ms:
            gMix = sb(ms, "gMix", [128, D], F32)
            gGrp = sb(ms, "gGrp", [128, D], F32)
            hT = sb(ms, "hT", [128, 8, SEQ], BF16)
            wsl = sb(ms, "wsl", [128, 8, 768], BF16)
            wout = sb(ms, "wout", [128, 8, D], BF16)
            Ttab = sb(ms, "Ttab", [128, 4, SEQ], BF16)
            TtabC = sb(ms, "TtabC", [128, 4, 256], BF16)
            qT = sb(ms, "qT", [128, 4, SEQ], BF16)
            kT = sb(ms, "kT", [128, 2, SEQ], BF16)
            kT32 = kT.t.bitcast(F32)
            kTv = [Tile(kT32[:, j, :], Buf(f"kTv{j}")) for j in range(2)]
            vaug = sb(ms, "vaug", [128, NT, 4, 65], BF16)
            yg = sb(ms, "yg", [128, NT, 256], F32)
            yT = sb(ms, "yT", [128, 8, SEQ], BF16)
            xts = sbn(ms, "xt", [128, D], F32, 2)
            xns = sbn(ms, "xn", [128, D], BF16, 2)
            junk = sb(ms, "junk", [128, D], BF16)
            Ebufs = sbn(ms, "Eb", [128, 512], BF16, 3)
            Pbufs = sbn(ms, "Pb", [128, 512], BF16, 4)
            Ubufs = sbn(ms, "Ub", [128, 512], F32, 2)
            Wbufs = sbn(ms, "Wb", [128, 512], BF16, 3)
            tmpc = sb(ms, "tmpc", [128, 4, 65], F32)
            Osb = sb(ms, "Osb", [128, 4, 65], F32)
            ssx = sb(ms, "ssx", [128, 2], F32)
            rsx = sb(ms, "rsx", [128, 2], F32)
            ssg = sb(ms, "ssg", [128, NT], F32)
            rsg = sb(ms, "rsg", [128, NT], F32)
            dden = sb(ms, "dden", [128, 4], F32)
            Rb = sb(ms, "Rb", [128, 4], F32)
            Cbs = sbn(ms, "Cb", [128, 4], F32, 2)
            gate = sb(ms, "gate", [128, 16], F32)
            top8 = sb(ms, "top8", [128, 8], F32)
            Msel = sb(ms, "Msel", [128, 4, NT, 8], F32)
            ksum = sb(ms, "ksum", [128, 2, 8], F32)
            ksumb = sb(ms, "ksumb", [128, 2, 8], BF16)

            load_gain(gMix, gvec["g_mix"], l)
            load_gain(gGrp, gvec["g_group"], l)
            S.dma("pool", wout.t[:], w_view(w_out_d, l, 0, D), w=[wout.b])
            S.dma("sp", TtabC.t[:], Texp.ap()[4:8, :, 0:256].rearrange("h p t -> p h t"), r=Texp_b[4:8], w=[TtabC.b])
            S.op("dve", lambda: dve.memset(vaug.t[:], 1.0), w=[vaug.b])
            S.op("pool", lambda: pool.memset(qT.t[:], 0.0), w=[qT.b])

            def load_wslice(col0, ncols):
                S.dma("pool", wsl.t[:, :, 0:ncols],
                      w_in_d.ap()[l, :, col0:col0 + ncols].rearrange("(c p) n -> p c n", p=128), w=[wsl.b])

            def proj_T(dst, nchunk, wcol0, scale, ei):
                return [(lambda cc=cc, tg=tg: proj_T_chunk(dst, cc, tg, wcol0, scale, ei)) for cc in range(nchunk) for tg in range(4)]

            def proj_T_chunk(dst, cc, tg, wcol0, scale, ei):
                if True:
                    if True:
                        pb = S.rot("psm", PS_M)
                        for c in range(8):
                            S.op("pe", lambda: pe.matmul(pb.t[:, :], lhsT=wsl.t[:, c, wcol0 + cc * 128: wcol0 + (cc + 1) * 128],
                                                         rhs=hT.t[:, c, tg * 512:(tg + 1) * 512], start=(c == 0), stop=(c == 7)),
                                 r=[wsl.b, hT.b], w=[pb.b])
                        ei[0] += 1
                        if dst is qT:
                            S.op("dve", lambda: dve.tensor_scalar(out=qT.t[0:64, 2 * cc, tg * 512:(tg + 1) * 512], in0=pb.t[0:64, :],
                                                                  scalar1=scale, scalar2=None, op0=ALU.mult),
                                 w=[pb.b, dst.b])
                            S.op("act", lambda: act.activation(out=qT.t[64:128, 2 * cc + 1, tg * 512:(tg + 1) * 512], in_=pb.t[64:128, :],
                                                               func=AF.Copy, scale=scale), w=[pb.b, dst.b])
                        elif ei[0] % 2 == 0:
                            S.op("dve", lambda: dve.tensor_scalar(out=dst.t[:, cc, tg * 512:(tg + 1) * 512], in0=pb.t[:, :],
                                                                  scalar1=scale, scalar2=None, op0=ALU.mult),
                                 w=[pb.b, dst.b, kTv[0].b, kTv[1].b])
                        else:
                            S.op("act", lambda: act.activation(out=dst.t[:, cc, tg * 512:(tg + 1) * 512], in_=pb.t[:, :],
                                                               func=AF.Copy, scale=scale), w=[pb.b, dst.b, kTv[0].b, kTv[1].b])

            def proj_V(wcol0, nh, ei):
                return [(lambda i=i: proj_V_chunk(wcol0, nh, ei, i)) for i in range(NT)]

            def proj_V_chunk(wcol0, nh, ei, i):
                if True:
                    pb = S.rot("psm", PS_M)
                    for c in range(8):
                        S.op("pe", lambda: pe.matmul(pb.t[:, 0:nh * 64], lhsT=hT.t[:, c, i * 128:(i + 1) * 128],
                                                     rhs=wsl.t[:, c, wcol0:wcol0 + nh * 64], start=(c == 0), stop=(c == 7)),
                             r=[wsl.b, hT.b], w=[pb.b])
                    srcv = pb.t[:, 0:nh * 64].rearrange("p (h e) -> p h e", e=64)
                    ei[0] += 1
                    if ei[0] % 2 == 0:
                        S.op("dve", lambda: dve.tensor_copy(out=vaug.t[:, i, 0:nh, 0:64], in_=srcv), w=[pb.b, vaug.b])
                    else:
                        S.op("act", lambda: act.copy(out=vaug.t[:, i, 0:nh, 0:64], in_=srcv), w=[pb.b, vaug.b])

            mulrr = [0]

            def mul_T(out_ap, in0_ap, in1_ap, r, w):
                mulrr[0] += 1
                if mulrr[0] % 2 == 0:
                    S.op("dve", lambda: dve.tensor_tensor(out=out_ap, in0=in0_ap, in1=in1_ap, op=ALU.mult), r=r, w=w)
                else:
                    S.op("pool", lambda: pool.tensor_tensor(out=out_ap, in0=in0_ap, in1=in1_ap, op=ALU.mult), r=r, w=w)

            def finish_group(g, Ob, h, extra=None, add_osb=False):
                Ov = Ob.t[:, 0:260].rearrange("p (j e) -> p j e", e=65)
                if add_osb:
                    S.op("dve", lambda: dve.tensor_tensor(out=Osb.t[:], in0=Ov, in1=Osb.t[:], op=ALU.add), w=[Ob.b, Osb.b])
                    src, srcb = Osb.t, Osb.b
                    den_ap = Osb.t[:, :, 64]
                else:
                    src, srcb = None, Ob.b
                    den_ap = Ov[:, :, 64]
                if extra is not None:
                    S.op("dve", lambda: dve.tensor_scalar(out=dden.t[:], in0=den_ap, scalar1=extra, scalar2=None, op0=ALU.add),
                         r=[expsink.b], w=[srcb, dden.b])
                    S.op("dve", lambda: dve.reciprocal(out=dden.t[:], in_=dden.t[:]), w=[dden.b])
                else:
                    S.op("dve", lambda: dve.reciprocal(out=dden.t[:], in_=den_ap), w=[srcb, dden.b])
                sap = Osb.t[:, :, 0:64] if add_osb else Ov[:, :, 0:64]
                dbc = bass.AP(dden.t, 0, [[4, 128], [1, 4], [0, 64]])
                S.op("dve", lambda: dve.tensor_tensor(out=yg.t[:, 4 * g:4 * g + 4, h * 64:(h + 1) * 64], in0=sap, in1=dbc, op=ALU.mult),
                     r=[dden.b], w=[srcb, yg.b])

            SB3 = [PS[0], PS[1], PS[4]]
            OB2 = [PS[2], PS[3]]
            PVB = [PS[5], PS[7]]

            def run_pipeline(steps, lags):
                n = len(steps)
                offs = [0]
                for lg in lags:
                    offs.append(offs[-1] + lg)
                for t in range(n + offs[-1]):
                    for si, o in enumerate(offs):
                        k = t - o
                        if 0 <= k < n:
                            steps[k][si]()

            def softmax_steps(h, kp, kc, qp, qc, vh, Tap, Tb_, window, extra=None):
                steps = []
                for g in range(4):
                    grp = {"Ob": None, "first": True}
                    bl = []
                    for b in range(max(0, 4 * g - window), 4 * g + 4):
                        jlo = max(0, b - 4 * g)
                        jhi = min(3, b + window - 4 * g)
                        if jlo <= jhi:
                            bl.append((b, jlo, jhi))
                    for idx, (b, jlo, jhi) in enumerate(bl):
                        st = {}
                        last = idx == len(bl) - 1

                        def F(st=st, b=b, jlo=jlo, jhi=jhi, g=g):
                            ncol = (jhi - jlo + 1) * 128
                            q0 = (4 * g + jlo) * 128
                            Sb_ = S.rot("sb3", SB3)
                            S.op("pe", lambda: pe.matmul(Sb_.t[:, 0:ncol], lhsT=kT.t[:, kc, b * 128:(b + 1) * 128],
                                                         rhs=qT.t[:, qc, q0:q0 + ncol], start=True, stop=True),
                                 r=[kT.b, qT.b], w=[Sb_.b])
                            e_ = S.rot("eb", Ebufs)
                            S.op("act", lambda: act.activation(out=e_.t[:, 0:ncol], in_=Sb_.t[:, 0:ncol], func=AF.Exp), w=[Sb_.b, e_.b])
                            p_ = S.rot("pb", Pbufs)
                            tau0 = q0 - 128 * b
                            S.op("dve", lambda: dve.tensor_tensor(out=p_.t[:, 0:ncol], in0=e_.t[:, 0:ncol], in1=Tap[:, tau0:tau0 + ncol], op=ALU.mult),
                                 r=[e_.b, Tb_], w=[p_.b])
                            st["p"] = p_

                        def B(st=st, b=b, jlo=jlo, jhi=jhi, g=g, grp=grp, last=last):
                            if grp["Ob"] is None:
                                grp["Ob"] = S.rot("ob2", OB2)
                            Ob = grp["Ob"]
                            p_ = st["p"]
                            for j in range(jlo, jhi + 1):
                                stf = grp["first"]
                                S.op("pe", lambda: pe.matmul(Ob.t[:, j * 65:(j + 1) * 65], lhsT=p_.t[:, (j - jlo) * 128:(j - jlo + 1) * 128],
                                                             rhs=vaug.t[:, b, vh, :], start=stf, stop=False, skip_group_check=True),
                                     r=[p_.b, vaug.b], w=[Ob.b])
                                grp["first"] = False
                            if last:
                                finish_group(g, Ob, h, extra=extra)

                        steps.append((F, B))
                return steps

            def stick_steps(h, kp, kc):
                steps = []
                for g in range(4):
                    for b in range(4 * g + 3, -1, -1):
                        st = {}
                        firstg = b == 4 * g + 3
                        lastg = b == 0

                        def S1(st=st, b=b, g=g):
                            jlo = max(0, b - 4 * g)
                            ncol = (4 - jlo) * 128
                            q0 = (4 * g + jlo) * 128
                            diag = b >= 4 * g
                            Zb = S.rot("sb3", SB3)
                            S.op("pe", lambda: pe.matmul(Zb.t[:, 0:ncol], lhsT=kT.t[:, kc, b * 128:(b + 1) * 128],
                                                         rhs=qT.t[:, h, q0:q0 + ncol], start=True, stop=False,
                                                         skip_group_check=True),
                                 r=[kT.b, qT.b], w=[Zb.b])
                            u_ = S.rot("ub", Ubufs)
                            S.op("act", lambda: act.activation(out=u_.t[:, 0:ncol], in_=Zb.t[:, 0:ncol], func=AF.Exp), w=[Zb.b, u_.b])
                            w_ = S.rot("wb", Wbufs)
                            S.op("act", lambda: act.activation(out=w_.t[:, 0:ncol], in_=u_.t[:, 0:ncol], func=AF.Ln, bias=1.0),
                                 r=[u_.b], w=[w_.b])
                            if diag:
                                S.op("pool", lambda: pool.tensor_tensor(out=w_.t[:, 0:128], in0=w_.t[:, 0:128], in1=maskb.t[:], op=ALU.mult),
                                     r=[maskb.b], w=[w_.b])
                            st.update(Zb=Zb, w=w_, jlo=jlo, ncol=ncol, diag=diag)

                        def S2(st=st):
                            Zb, w_, ncol = st["Zb"], st["w"], st["ncol"]
                            S.op("pe", lambda: pe.matmul(Zb.t[:, 0:ncol], lhsT=trineg.t[:], rhs=w_.t[:, 0:ncol], start=False, stop=True,
                                                         skip_group_check=True),
                                 r=[trineg.b, w_.b], w=[Zb.b])
                            p_ = S.rot("pb", Pbufs)
                            S.op("act", lambda: act.activation(out=p_.t[:, 0:ncol], in_=Zb.t[:, 0:ncol], func=AF.Exp), w=[Zb.b, p_.b])
                            if st["diag"]:
                                S.op("pool", lambda: pool.tensor_tensor(out=p_.t[:, 0:128], in0=p_.t[:, 0:128], in1=maskb.t[:], op=ALU.mult),
                                     r=[maskb.b], w=[p_.b])
                            st["p"] = p_

                        def S3(st=st, b=b, g=g, firstg=firstg, lastg=lastg):
                            p_, w_, jlo = st["p"], st["w"], st["jlo"]
                            if firstg:
                                S.op("dve", lambda: dve.memset(Osb.t[:], 0.0), w=[Osb.b])
                                S.op("dve", lambda: dve.memset(Rb.t[:], 0.0), w=[Rb.b])
                                S.op("dve", lambda: dve.memset(Cbs[0].t[:], 1.0), w=[Cbs[0].b])
                                S.op("dve", lambda: dve.memset(Cbs[1].t[:], 1.0), w=[Cbs[1].b])
                            kk = (4 * g + 3 - b) % 2
                            Cc, Cn = Cbs[kk], Cbs[1 - kk]
                            Pv = S.rot("pvb", PVB)
                            first = True
                            for j in range(jlo, 4):
                                stf = first
                                S.op("pe", lambda: pe.matmul(Pv.t[:, j * 64:(j + 1) * 64], lhsT=p_.t[:, (j - jlo) * 128:(j - jlo + 1) * 128],
                                                             rhs=vaug.t[:, b, h, 0:64], start=stf, stop=False, skip_group_check=True),
                                     r=[p_.b, vaug.b], w=[Pv.b])
                                first = False
                                if not lastg:
                                    S.op("pe", lambda: pe.matmul(Pv.t[:, 256 + j:257 + j], lhsT=w_.t[:, (j - jlo) * 128:(j - jlo + 1) * 128],
                                                                 rhs=onescol.t[:, 0:1], start=False, stop=False, skip_group_check=True),
                                         r=[w_.b, onescol.b], w=[Pv.b])
                            if not lastg:
                                S.op("dve", lambda: dve.tensor_tensor(out=Rb.t[:, jlo:4], in0=Pv.t[:, 256 + jlo:260], in1=Rb.t[:, jlo:4], op=ALU.add),
                                     w=[Pv.b, Rb.b])
                                S.op("act", lambda: act.activation(out=Cn.t[:, jlo:4], in_=Rb.t[:, jlo:4], func=AF.Exp, scale=-1.0),
                                     r=[Rb.b], w=[Cn.b])
                            nj = 4 - jlo
                            cbc = bass.AP(Cc.t, jlo, [[4, 128], [1, nj], [0, 64]])
                            Pv3 = Pv.t[:, jlo * 64:256].rearrange("p (j e) -> p j e", e=64)
                            S.op("dve", lambda: dve.tensor_tensor(out=tmpc.t[:, jlo:4, 0:64], in0=Pv3, in1=cbc, op=ALU.mult),
                                 r=[Cc.b], w=[Pv.b, tmpc.b])
                            S.op("dve", lambda: dve.tensor_tensor(out=Osb.t[:, jlo:4, 0:64], in0=tmpc.t[:, jlo:4, 0:64], in1=Osb.t[:, jlo:4, 0:64],
                                                                  op=ALU.add),
                                 r=[tmpc.b], w=[Osb.b])
                            if lastg:
                                S.op("pool", lambda: pool.tensor_copy(out=yg.t[:, 4 * g:4 * g + 4, h * 64:(h + 1) * 64], in_=Osb.t[:, :, 0:64]),
                                     r=[Osb.b], w=[yg.b])

                        steps.append((S1, S2, S3))
                return steps

            def moba_gates(h, kp, kc):
                for i in range(8, NT):
                    qb = i // 2
                    S.op("pe", lambda: pe.matmul(PS[6].t[:, 0:8], lhsT=qT.t[:, h, i * 128:(i + 1) * 128],
                                                 rhs=ksumb.t[:, kc, :], start=True, stop=True),
                         r=[qT.b, ksumb.b], w=[PS[6].b])
                    S.op("dve", lambda: dve.memset(gate.t[:], -BIG), w=[gate.b])
                    S.op("dve", lambda: dve.tensor_copy(out=gate.t[:, 0:qb], in_=PS[6].t[:, 0:qb]), w=[PS[6].b, gate.b])
                    S.op("dve", lambda: dve.max(out=top8.t[:], in_=gate.t[:]), r=[gate.b], w=[top8.b])
                    S.op("dve", lambda: dve.tensor_scalar(out=Msel.t[:, h, i, :], in0=gate.t[:, 0:8], scalar1=top8.t[:, 2:3], scalar2=None,
                                                          op0=ALU.is_ge),
                         r=[gate.b, top8.b], w=[Msel.b])

            def moba_steps(h, kp, kc):
                Tap = Ttab.t[:, h, :]
                steps = []
                for g in range(4):
                    grp = {"Own": None, "first_own": True, "Pvp": None, "first_pv": True}
                    for b in range(0, 4 * g + 4):
                        st = {}
                        firstg = b == 0
                        lastg = b == 4 * g + 3

                        def F(st=st, b=b, g=g):
                            jlo = max(0, b - 4 * g)
                            ncol = (4 - jlo) * 128
                            q0 = (4 * g + jlo) * 128
                            Sb_ = S.rot("sb3", SB3)
                            S.op("pe", lambda: pe.matmul(Sb_.t[:, 0:ncol], lhsT=kT.t[:, kc, b * 128:(b + 1) * 128],
                                                         rhs=qT.t[:, h, q0:q0 + ncol], start=True, stop=True),
                                 r=[kT.b, qT.b], w=[Sb_.b])
                            e_ = S.rot("eb", Ebufs)
                            S.op("act", lambda: act.activation(out=e_.t[:, 0:ncol], in_=Sb_.t[:, 0:ncol], func=AF.Exp), w=[Sb_.b, e_.b])
                            p_ = S.rot("pb", Pbufs)
                            tau0 = q0 - 128 * b
                            S.op("dve", lambda: dve.tensor_tensor(out=p_.t[:, 0:ncol], in0=e_.t[:, 0:ncol], in1=Tap[:, tau0:tau0 + ncol], op=ALU.mult),
                                 r=[e_.b, Ttab.b], w=[p_.b])
                            st.update(p=p_, jlo=jlo)

                        def B(st=st, b=b, g=g, grp=grp, firstg=firstg, lastg=lastg):
                            p_, jlo = st["p"], st["jlo"]
                            n = b // 2
                            if firstg:
                                grp["Own"] = S.rot("ob2", OB2)
                                S.op("dve", lambda: dve.memset(Osb.t[:], 0.0), w=[Osb.b])
                            if b % 2 == 0:
                                grp["Pvp"] = S.rot("pvb", PVB)
                                grp["first_pv"] = True
                            Own, Pvp = grp["Own"], grp["Pvp"]
                            for j in range(jlo, 4):
                                qb = (4 * g + j) // 2
                                if n == qb or g < 2:
                                    stf = grp["first_own"]
                                    S.op("pe", lambda: pe.matmul(Own.t[:, j * 65:(j + 1) * 65], lhsT=p_.t[:, (j - jlo) * 128:(j - jlo + 1) * 128],
                                                                 rhs=vaug.t[:, b, h, :], start=stf, stop=False, skip_group_check=True),
                                         r=[p_.b, vaug.b], w=[Own.b])
                                    grp["first_own"] = False
                                else:
                                    stf = grp["first_pv"]
                                    S.op("pe", lambda: pe.matmul(Pvp.t[:, j * 65:(j + 1) * 65], lhsT=p_.t[:, (j - jlo) * 128:(j - jlo + 1) * 128],
                                                                 rhs=vaug.t[:, b, h, :], start=stf, stop=False, skip_group_check=True),
                                         r=[p_.b, vaug.b], w=[Pvp.b])
                                    grp["first_pv"] = False
                            if b % 2 == 1 and g >= 2:
                                jA = 0 if n < 2 * g else 2
                                if n < 2 * g + 1:
                                    nj = 4 - jA
                                    Mbc = bass.AP(Msel.t, ((h * NT + 4 * g + jA) * 8 + n), [[4 * NT * 8, 128], [8, nj], [0, 65]])
                                    Pv3 = Pvp.t[:, jA * 65:4 * 65].rearrange("p (j e) -> p j e", e=65)
                                    S.op("dve", lambda: dve.tensor_tensor(out=tmpc.t[:, jA:4, :], in0=Pv3, in1=Mbc, op=ALU.mult),
                                         r=[Msel.b], w=[Pvp.b, tmpc.b])
                                    S.op("dve", lambda: dve.tensor_tensor(out=Osb.t[:, jA:4, :], in0=tmpc.t[:, jA:4, :], in1=Osb.t[:, jA:4, :],
                                                                          op=ALU.add),
                                         r=[tmpc.b], w=[Osb.b])
                            if lastg:
                                finish_group(g, Own, h, add_osb=True)

                        steps.append((F, B))
                return steps

            def group_norm(m):
                pieces = []

                def sq(i):
                    def f():
                        S.op("act", lambda: act.activation(out=junk.t[:, 0:256], in_=yg.t[:, i, :], func=AF.Square, accum_out=ssg.t[:, i:i + 1]),
                             r=[yg.b], w=[junk.b, ssg.b])
                    return f

                def stats():
                    S.op("act", lambda: act.activation(out=rsg.t[:], in_=ssg.t[:], func=AF.Ln, scale=1.0 / 256, bias=epsc.t[:, 0:1]),
                         r=[ssg.b, epsc.b], w=[rsg.b])
                    S.op("act", lambda: act.activation(out=rsg.t[:], in_=rsg.t[:], func=AF.Exp, scale=-0.5), w=[rsg.b])

                def tr(i):
                    def f():
                        xn = S.rot("xn", xns)
                        S.op("dve", lambda: dve.scalar_tensor_tensor(out=xn.t[:, 0:256], in0=yg.t[:, i, :], scalar=rsg.t[:, i:i + 1],
                                                                     in1=gGrp.t[:, m * 256:(m + 1) * 256], op0=ALU.mult, op1=ALU.mult),
                             r=[yg.b, rsg.b, gGrp.b], w=[xn.b])
                        transpose_to(yT.t[:, 2 * m:2 * m + 2, i * 128:(i + 1) * 128], yT.b, xn, 2, eng=("dve" if i % 2 else "act"))
                    return f

                for i in range(0, NT, 4):
                    pieces.append(lambda i=i: [sq(j)() for j in range(i, i + 4)])
                pieces.append(stats)
                for i in range(NT):
                    pieces.append(tr(i))
                return pieces

            def interleave(main, side, every=2):
                si = 0
                for ci, ch in enumerate(main):
                    ch()
                    if ci % every == every - 1 and si < len(side):
                        side[si]()
                        si += 1
                while si < len(side):
                    side[si]()
                    si += 1

            for s in range(0 if _os.environ.get("T_SKIPMIX") else n_seq):
                for i in range(NT):
                    xt = S.rot("xt", xts)
                    S.dma("sp", xt.t[:], x_src.ap()[s, i * 128:(i + 1) * 128, :],
                          r=([x_src_b[s][i]] if x_src_b else none_b), w=[xt.b])
                    k2 = i % 2
                    rms_rstd(xt.t[:], D, junk, ssx.t[:, k2:k2 + 1], ssx.b, rsx.t[:, k2:k2 + 1], rsx.b, xt.b)
                    xn = S.rot("xn", xns)
                    S.op("dve", lambda: dve.scalar_tensor_tensor(out=xn.t[:], in0=xt.t[:], scalar=rsx.t[:, k2:k2 + 1], in1=gMix.t[:],
                                                                 op0=ALU.mult, op1=ALU.mult),
                         r=[xt.b, rsx.b, gMix.b], w=[xn.b])
                    transpose_to(hT.t[:, :, i * 128:(i + 1) * 128], hT.b, xn, 8, eng=("dve" if i % 2 else "act"))

                ei = [0]
                load_wslice(A_Q, 768)
                S.dma("sp", Ttab.t[:], Texp.ap()[0:4].rearrange("h p t -> p h t"), r=Texp_b[0:4], w=[Ttab.b])
                interleave(proj_T(qT, 2, 0, 0.125, ei) + proj_T(kT, 2, 256, 1.0, ei) + proj_V(512, 4, ei), [])
                load_wslice(B_Q, 768)
                steps = []
                for h in range(4):
                    steps += softmax_steps(h, (h % 2) * 64, h // 2, (h % 2) * 64, h, h, Ttab.t[:, h, :], Ttab.b, 15)
                run_pipeline(steps, [2])
                S.dma("sp", Ttab.t[:], Texp.ap()[8:12].rearrange("h p t -> p h t"), r=Texp_b[8:12], w=[Ttab.b])
                gn_pending = group_norm(0)
                if dbg and s == 0 and l == 0:
                    S.dma("sp", dbg_out["yg"].ap()[0].rearrange("(i p) f -> p i f", p=128), yg.t[:], r=[yg.b])
                interleave(proj_T(qT, 2, 0, 0.125, ei) + proj_T(kT, 2, 256, 1.0, ei) + proj_V(512, 4, ei), gn_pending)
                load_wslice(C_Q, 512)
                steps = []
                for h in range(4):
                    steps += stick_steps(h, (h % 2) * 64, h // 2)
                run_pipeline(steps, [1, 1])
                gn_pending = group_norm(1)
                if dbg and s == 0 and l == 0:
                    S.dma("sp", dbg_out["yg"].ap()[1].rearrange("(i p) f -> p i f", p=128), yg.t[:], r=[yg.b])
                interleave(proj_T(qT, 2, 0, 0.125, ei) + proj_T(kT, 1, 256, 1.0, ei) + proj_V(384, 2, ei), gn_pending)
                load_wslice(D_Q, 768)
                steps = []
                for h in range(4):
                    kvh = h // 2
                    qc = {0: 0, 2: 1, 1: 2, 3: 3}[h]
                    steps += softmax_steps(h, kvh * 64, 0, kvh * 64, qc, kvh, TtabC.t[:, h, :], TtabC.b, 1,
                                           extra=expsink.t[:, l * 4 + h:l * 4 + h + 1])
                run_pipeline(steps, [2])
                gn_pending = group_norm(2)
                if dbg and s == 0 and l == 0:
                    S.dma("sp", dbg_out["yg"].ap()[2].rearrange("(i p) f -> p i f", p=128), yg.t[:], r=[yg.b])
                interleave(proj_T(qT, 2, 0, 0.125, ei) + proj_T(kT, 2, 256, 1.0, ei) + proj_V(512, 4, ei), gn_pending)
                S.op("dve", lambda: dve.tensor_reduce(out=ksum.t[:], in_=kT.t[:].rearrange("p c (n k) -> p c n k", k=256),
                                                      axis=mybir.AxisListType.X, op=ALU.add),
                     r=[kT.b], w=[ksum.b])
                S.op("dve", lambda: dve.tensor_copy(out=ksumb.t[:], in_=ksum.t[:]), r=[ksum.b], w=[ksumb.b])
                steps = []
                for h in range(4):
                    moba_gates(h, (h % 2) * 64, h // 2)
                for h in range(4):
                    steps += moba_steps(h, (h % 2) * 64, h // 2)
                run_pipeline(steps, [2])
                interleave([], group_norm(3))
                if dbg and s == 0 and l == 0:
                    S.dma("sp", dbg_out["yg"].ap()[3].rearrange("(i p) f -> p i f", p=128), yg.t[:], r=[yg.b])
                OPB = [PS[4], PS[5], PS[0], PS[1]]
                xt_of = {}
                xt_out = [xts[0], xts[1], kTv[0], kTv[1]]

                def load_x(i):
                    xt_ = S.rot("xto", xt_out)
                    extra_w = [kT.b] if (xt_ is kTv[0] or xt_ is kTv[1]) else []
                    S.dma("sp", xt_.t[:], x_src.ap()[s, i * 128:(i + 1) * 128, :],
                          r=([x_src_b[s][i]] if x_src_b else none_b), w=[xt_.b] + extra_w)
                    xt_of[i] = xt_

                load_x(0)
                for i in range(NT):
                    if i + 1 < NT:
                        load_x(i + 1)
                    xt = xt_of[i]
                    for hf in range(2):
                        pb = S.rot("opb", OPB)
                        for c in range(8):
                            S.op("pe", lambda: pe.matmul(pb.t[:, :], lhsT=yT.t[:, c, i * 128:(i + 1) * 128],
                                                         rhs=wout.t[:, c, hf * 512:(hf + 1) * 512], start=(c == 0), stop=(c == 7)),
                                 r=[yT.b, wout.b], w=[pb.b])
                        S.op("dve", lambda: dve.tensor_tensor(out=xt.t[:, hf * 512:(hf + 1) * 512], in0=pb.t[:, :],
                                                              in1=xt.t[:, hf * 512:(hf + 1) * 512], op=ALU.add),
                             w=[pb.b, xt.b])
                    S.dma("sp", xA.ap()[s, i * 128:(i + 1) * 128, :], xt.t[:], r=[xt.b], w=[xA_b[s][i]])
                    if dbg and l == 0:
                        S.dma("sp", dbg_out["x1"].ap()[s, i * 128:(i + 1) * 128, :], xt.t[:], r=[xt.b])
            S.barrier()

        with contextlib.ExitStack() as ks:
            gMem = sb(ks, "gMem", [128, D], F32)
            wxkv = sb(ks, "wxkv", [128, 8, 512], BF16)
            mT = sb(ks, "mT", [128, 8, MEM], BF16)
            xts = sbn(ks, "mxt", [128, D], F32, 2)
            xns = sbn(ks, "mxn", [128, D], BF16, 2)
            junk = sb(ks, "mjunk", [128, D], BF16)
            ssx = sb(ks, "mssx", [128, 2], F32)
            rsx = sb(ks, "mrsx", [128, 2], F32)
            load_gain(gMem, gvec["g_mem"], l)
            S.dma("pool", wxkv.t[:], w_view(w_xkv_d, l, 0, D), w=[wxkv.b])
            S.op("dve", lambda: dve.memset(vxa.t[:], 1.0), w=[vxa.b])
            for s in range(n_seq):
                for i in range(2):
                    xt = S.rot("mxt", xts)
                    S.dma("sp", xt.t[:], mem_in.ap()[s, i * 128:(i + 1) * 128, :], w=[xt.b])
                    rms_rstd(xt.t[:], D, junk, ssx.t[:, i:i + 1], ssx.b, rsx.t[:, i:i + 1], rsx.b, xt.b)
                    xn = S.rot("mxn", xns)
                    S.op("dve", lambda: dve.scalar_tensor_tensor(out=xn.t[:], in0=xt.t[:], scalar=rsx.t[:, i:i + 1], in1=gMem.t[:],
                                                                 op0=ALU.mult, op1=ALU.mult),
                         r=[xt.b, rsx.b, gMem.b], w=[xn.b])
                    transpose_to(mT.t[:, :, i * 128:(i + 1) * 128], mT.b, xn, 8)
                for cc in range(2):
                    pb = S.rot("psm", PS_M)
                    for c in range(8):
                        S.op("pe", lambda: pe.matmul(pb.t[:, 0:MEM], lhsT=wxkv.t[:, c, cc * 128:(cc + 1) * 128], rhs=mT.t[:, c, :],
                                                     start=(c == 0), stop=(c == 7)),
                             r=[wxkv.b, mT.b], w=[pb.b])
                    S.op("dve", lambda: dve.tensor_copy(out=kxT.t[:, s, cc, :], in_=pb.t[:, 0:MEM]), w=[pb.b, kxT.b])
                for i in range(2):
                    pb = S.rot("psm", PS_M)
                    for c in range(8):
                        S.op("pe", lambda: pe.matmul(pb.t[:, 0:256], lhsT=mT.t[:, c, i * 128:(i + 1) * 128], rhs=wxkv.t[:, c, 256:512],
                                                     start=(c == 0), stop=(c == 7)),
                             r=[wxkv.b, mT.b], w=[pb.b])
                    S.op("dve", lambda: dve.tensor_copy(out=vxa.t[:, s, i, :, 0:64],
                                                        in_=pb.t[:, 0:256].rearrange("p (h e) -> p h e", e=64)),
                         w=[pb.b, vxa.b])
            S.barrier()

        with contextlib.ExitStack() as ts:
            gFin = sb(ts, "gFin", [128, D], F32) if last else None
            gCT = sb(ts, "gCT", [128, 8], F32)
            gMT = sb(ts, "gMT", [128, 8], F32)
            wup = sb(ts, "wup", [128, 8, DFF], BF16)
            wdn = sb(ts, "wdn", [128, 32, D], BF16)
            wxq = sb(ts, "wxq", [128, 8, 256], BF16)
            wxo = sb(ts, "wxo", [128, 2, D], BF16)
            xgs = sbn(ts, "xg", [128, 2, D], F32, 2)
            xnk = sbn(ts, "txn", [128, D], BF16, 2)
            hTc = sb(ts, "hTc", [128, 8, 256], BF16)
            hTms = sbn(ts, "hTm", [128, 8, 256], BF16, 2)
            qxT = sb(ts, "qxT", [128, 4, 256], BF16)
            Eb = sbn(ts, "tEb", [128, 512], BF16, 2)
            oxn = sb(ts, "oxn", [128, 256], BF16)
            oxT = sb(ts, "oxT", [128, 2, 256], BF16)
            aT = sb(ts, "aT", [128, 32, 256], BF16)
            ssx = sb(ts, "tssx", [128, 2], F32)
            ssf = sb(ts, "tssf", [128, 2], F32)
            rsf = sb(ts, "trsf", [128, 2], F32)
            fjunk = sb(ts, "fjunk", [128, D], BF16)
            rsx = sb(ts, "trsx", [128, 2], F32)
            dden = sb(ts, "tdden", [128, 4], F32)
            rl = sbn(ts, "rl", [128, 256], F32, 2)
            S.op("pool", lambda: pool.memset(qxT.t[:], 0.0), w=[qxT.b])
            S.dma("sp", gCT.t[:], gT_in["gT_cross"].ap()[l], w=[gCT.b])
            S.dma("sp", gMT.t[:], gT_in["gT_mlp"].ap()[l], w=[gMT.b])
            if last:
                load_gain(gFin, gfin_in, 0)
            S.dma("pool", wxq.t[:], w_view(w_xq_d, l, 0, D), w=[wxq.b])
            S.dma("pool", wxo.t[:], w_view(w_xo_d, l, 0, 256), w=[wxo.b])
            for c4 in range(4):
                S.dma("pool", wup.t[:, :, c4 * 1024:(c4 + 1) * 1024],
                      w_up_d.ap()[l, :, c4 * 1024:(c4 + 1) * 1024].rearrange("(c p) n -> p c n", p=128), w=[wup.b])
            for c4 in range(4):
                S.dma("pool", wdn.t[:, c4 * 8:(c4 + 1) * 8, :], w_view(w_dn_d, l, c4 * 1024, 1024), w=[wdn.b])

            FS = [PS[0], PS[1]]
            FO = PS[2]
            UPB = [PS[4], PS[5]]
            DNB = [PS[3], PS[7]]
            groups = [(s, tg) for s in range(n_seq) for tg in range(SEQ // 256)]

            def norm_stage(xg, k, xn):
                def f():
                    S.op("act", lambda: act.activation(out=xn.t[:], in_=xg.t[:, k, :], func=AF.Square, accum_out=ssx.t[:, k:k + 1]),
                         r=[xg.b], w=[xn.b, ssx.b])
                    S.op("act", lambda: act.activation(out=rsx.t[:, k:k + 1], in_=ssx.t[:, k:k + 1], func=AF.Ln, scale=1.0 / D,
                                                       bias=epsc.t[:, 0:1]), r=[ssx.b, epsc.b], w=[rsx.b])
                    S.op("act", lambda: act.activation(out=rsx.t[:, k:k + 1], in_=rsx.t[:, k:k + 1], func=AF.Exp, scale=-0.5),
                         w=[rsx.b])
                    S.op("dve", lambda: dve.tensor_scalar(out=xn.t[:], in0=xg.t[:, k, :], scalar1=rsx.t[:, k:k + 1], scalar2=None,
                                                          op0=ALU.mult), r=[xg.b, rsx.b], w=[xn.b])
                return f

            def transp_stage(xn, dst, k, gT):
                def f():
                    pb = PS[6].t.bitcast(BF16)
                    for c in range(8):
                        S.op("pe", lambda: pe.transpose(out=pb[:, c * 128:(c + 1) * 128], in_=xn.t[:, c * 128:(c + 1) * 128],
                                                        identity=ident.t[:]), r=[xn.b, ident.b], w=[PS[6].b])
                    for c in range(8):
                        if c % 2 == 0 or _os.environ.get('T_NOACT'):
                            S.op("dve", lambda: dve.tensor_scalar(out=dst.t[:, c, k * 128:(k + 1) * 128], in0=pb[:, c * 128:(c + 1) * 128],
                                                                  scalar1=gT.t[:, c:c + 1], scalar2=None, op0=ALU.mult),
                                 r=[gT.b], w=[PS[6].b, dst.b])
                        else:
                            S.op("act", lambda: act.activation(out=dst.t[:, c, k * 128:(k + 1) * 128], in_=pb[:, c * 128:(c + 1) * 128],
                                                               func=AF.Identity, scale=gT.t[:, c:c + 1]),
                                 r=[gT.b], w=[PS[6].b, dst.b])
                return f

            def make_F(t):
                s, tg = groups[t]
                xg = xgs[t % 2]
                hTm = hTms[t % 2]
                st = []

                def load(k):
                    def f():
                        i = tg * 2 + k
                        S.dma("sp", xg.t[:, k, :], xA.ap()[s, i * 128:(i + 1) * 128, :], r=[xA_b[s][i]], w=[xg.b])
                    return f

                def qproj():
                    for cc in range(2):
                        for c in range(8):
                            S.op("pe", lambda: pe.matmul(FO.t[:, cc * 256:(cc + 1) * 256], lhsT=wxq.t[:, c, cc * 128:(cc + 1) * 128],
                                                         rhs=hTc.t[:, c, :], start=(c == 0 and cc == 0), stop=(c == 7),
                                                         skip_group_check=True),
                                 r=[wxq.b, hTc.b], w=[FO.b])
                    FOv = FO.t[:, :].rearrange("p (c t) -> p c t", t=256)
                    S.op("dve", lambda: dve.tensor_scalar(out=qxT.t[0:64, 0:4:2, :], in0=FOv[0:64, :, :],
                                                          scalar1=0.125, scalar2=None, op0=ALU.mult), w=[FO.b, qxT.b])
                    S.op("dve", lambda: dve.tensor_scalar(out=qxT.t[64:128, 1:4:2, :], in0=FOv[64:128, :, :],
                                                          scalar1=0.125, scalar2=None, op0=ALU.mult), w=[FO.b, qxT.b])

                def scores(k):
                    def f():
                        for idx in range(8):
                            h, mt = idx // 2, idx % 2
                            hp, hc = (h % 2) * 64, h // 2
                            bank = FS[h % 2]
                            col = ((h // 2) * 2 + mt) * 128
                            S.op("pe", lambda: pe.matmul(bank.t[:, col:col + 128], lhsT=kxT.t[:, s, hc, mt * 128:(mt + 1) * 128],
                                                         rhs=qxT.t[:, h, k * 128:(k + 1) * 128], start=True, stop=True,
                                                         skip_group_check=True),
                                 r=[kxT.b, qxT.b], w=[bank.b])
                        for bi in range(2):
                            S.op("act", lambda: act.activation(out=Eb[bi].t[:, :], in_=FS[bi].t[:, :], func=AF.Exp), w=[FS[bi].b, Eb[bi].b])
                    return f

                def pv(k):
                    def f():
                        first = True
                        for idx in range(8):
                            h, mt = idx // 2, idx % 2
                            e_ = Eb[h % 2]
                            col = ((h // 2) * 2 + mt) * 128
                            stf = first
                            S.op("pe", lambda: pe.matmul(FO.t[:, h * 65:(h + 1) * 65], lhsT=e_.t[:, col:col + 128], rhs=vxa.t[:, s, mt, h, :],
                                                         start=stf, stop=False, skip_group_check=True),
                                 r=[e_.b, vxa.b], w=[FO.b])
                            first = False
                        Ov = FO.t[:, 0:260].rearrange("p (j e) -> p j e", e=65)
                        S.op("dve", lambda: dve.reciprocal(out=dden.t[:], in_=Ov[:, :, 64]), w=[FO.b, dden.b])
                        for h in range(4):
                            S.op("dve", lambda: dve.tensor_scalar(out=oxn.t[:, h * 64:(h + 1) * 64], in0=Ov[:, h, 0:64],
                                                                  scalar1=dden.t[:, h:h + 1], scalar2=None, op0=ALU.mult),
                                 r=[dden.b], w=[FO.b, oxn.b])
                        pb = PS[6].t.bitcast(BF16)
                        for c in range(2):
                            S.op("pe", lambda: pe.transpose(out=pb[:, c * 128:(c + 1) * 128], in_=oxn.t[:, c * 128:(c + 1) * 128],
                                                            identity=ident.t[:]), r=[oxn.b, ident.b], w=[PS[6].b])
                        S.op("dve", lambda: dve.tensor_copy(out=oxT.t[:, :, k * 128:(k + 1) * 128],
                                                            in_=pb[:, 0:256].rearrange("p (c t) -> p c t", t=128)),
                             w=[PS[6].b, oxT.b])
                    return f

                def oproj(k):
                    def f():
                        for hf in range(2):
                            for c in range(2):
                                S.op("pe", lambda: pe.matmul(FO.t[:, :], lhsT=oxT.t[:, c, k * 128:(k + 1) * 128],
                                                             rhs=wxo.t[:, c, hf * 512:(hf + 1) * 512], start=(c == 0), stop=(c == 1)),
                                     r=[oxT.b, wxo.b], w=[FO.b])
                            S.op("dve", lambda: dve.tensor_tensor(out=xg.t[:, k, hf * 512:(hf + 1) * 512], in0=FO.t[:, :],
                                                                  in1=xg.t[:, k, hf * 512:(hf + 1) * 512], op=ALU.add),
                                 w=[FO.b, xg.b])
                    return f

                st.append(load(0))
                st.append(load(1))
                st.append(norm_stage(xg, 0, xnk[0]))
                st.append(norm_stage(xg, 1, xnk[1]))
                st.append(transp_stage(xnk[0], hTc, 0, gCT))
                st.append(transp_stage(xnk[1], hTc, 1, gCT))
                st.append(qproj)
                st.append(scores(0))
                st.append(pv(0))
                st.append(scores(1))
                st.append(pv(1))
                st.append(oproj(0))
                st.append(oproj(1))
                st.append(norm_stage(xg, 0, xnk[0]))
                st.append(norm_stage(xg, 1, xnk[1]))
                st.append(transp_stage(xnk[0], hTm, 0, gMT))
                st.append(transp_stage(xnk[1], hTm, 1, gMT))
                return st

            def make_M(t):
                s, tg = groups[t]
                xg = xgs[t % 2]
                hTm = hTms[t % 2]
                ch = []

                def up(fc):
                    def f():
                        pb = S.rot("upb", UPB)
                        for c in range(8):
                            S.op("pe", lambda: pe.matmul(pb.t[:, 0:256], lhsT=wup.t[:, c, fc * 128:(fc + 1) * 128], rhs=hTm.t[:, c, :],
                                                         start=(c == 0), stop=(c == 7)),
                                 r=[wup.b, hTm.b], w=[pb.b])
                        r_ = S.rot("rl", rl)
                        if fc % 2 == 0:
                            S.op("act", lambda: act.activation(out=r_.t[:, :], in_=pb.t[:, 0:256], func=AF.Relu), w=[pb.b, r_.b])
                            S.op("pool", lambda: pool.tensor_tensor(out=aT.t[:, fc, :], in0=r_.t[:, :], in1=r_.t[:, :], op=ALU.mult),
                                 r=[r_.b], w=[aT.b])
                        else:
                            S.op("dve", lambda: dve.tensor_scalar(out=r_.t[:, :], in0=pb.t[:, 0:256], scalar1=0.0, scalar2=None, op0=ALU.max),
                                 w=[pb.b, r_.b])
                            S.op("dve", lambda: dve.tensor_tensor(out=aT.t[:, fc, :], in0=r_.t[:, :], in1=r_.t[:, :], op=ALU.mult),
                                 r=[r_.b], w=[aT.b])
                    return f

                def down(k, hf):
                    def f():
                        pb = S.rot("dnb", DNB)
                        for fc in range(32):
                            S.op("pe", lambda: pe.matmul(pb.t[:, :], lhsT=aT.t[:, fc, k * 128:(k + 1) * 128],
                                                         rhs=wdn.t[:, fc, hf * 512:(hf + 1) * 512], start=(fc == 0), stop=(fc == 31)),
                                 r=[aT.b, wdn.b], w=[pb.b])
                        S.op("dve", lambda: dve.tensor_tensor(out=xg.t[:, k, hf * 512:(hf + 1) * 512], in0=pb.t[:, :],
                                                              in1=xg.t[:, k, hf * 512:(hf + 1) * 512], op=ALU.add),
                             w=[pb.b, xg.b])
                        if hf == 1:
                            i = tg * 2 + k
                            if last:
                                S.op("act", lambda: act.activation(out=fjunk.t[:], in_=xg.t[:, k, :], func=AF.Square, accum_out=ssf.t[:, k:k + 1]),
                                     r=[xg.b], w=[fjunk.b, ssf.b])
                                S.op("act", lambda: act.activation(out=rsf.t[:, k:k + 1], in_=ssf.t[:, k:k + 1], func=AF.Ln, scale=1.0 / D,
                                                                   bias=epsc.t[:, 0:1]), r=[ssf.b, epsc.b], w=[rsf.b])
                                S.op("act", lambda: act.activation(out=rsf.t[:, k:k + 1], in_=rsf.t[:, k:k + 1], func=AF.Exp, scale=-0.5),
                                     w=[rsf.b])
                                S.op("dve", lambda: dve.scalar_tensor_tensor(out=xg.t[:, k, :], in0=xg.t[:, k, :], scalar=rsf.t[:, k:k + 1],
                                                                             in1=gFin.t[:], op0=ALU.mult, op1=ALU.mult),
                                     r=[rsf.b, gFin.b], w=[xg.b])
                                S.dma("sp", y_out.ap()[s, i * 128:(i + 1) * 128, :], xg.t[:, k, :], r=[xg.b], w=[y_b[s][i]])
                            else:
                                S.dma("sp", xB.ap()[s, i * 128:(i + 1) * 128, :], xg.t[:, k, :], r=[xg.b], w=[xB_b[s][i]])
                    return f

                for fc in range(32):
                    ch.append(up(fc))
                for k in range(2):
                    for hf in range(2):
                        ch.append(down(k, hf))
                return ch

            _stop = int(_os.environ.get("T_STOP", "999"))
            for f in make_F(0)[:_stop]:
                f()
            for t in range(len(groups) if _stop >= 999 else 0):
                Mt = make_M(t)
                Fn = make_F(t + 1) if t + 1 < len(groups) else []
                fi = 0
                for ci, chunk in enumerate(Mt):
                    chunk()
                    if ci % 2 == 1 and fi < len(Fn) and not _os.environ.get('T_NOINTER'):
                        Fn[fi]()
                        fi += 1
                while fi < len(Fn):
                    Fn[fi]()
                    fi += 1
            S.barrier()
    S.barrier()
    return nc


_C_PERM = np.concatenate([np.arange(0, 64), np.arange(128, 192), np.arange(64, 128), np.arange(192, 256)])


def make_in_maps(inputs, n_cores, n_seq):
    f32 = np.float32
    w_in = np.asarray(inputs["w_in"], f32)
    perm = np.arange(IN_W)
    perm[C_Q:C_Q + 256] = C_Q + _C_PERM
    shared = {
        "rel_table": np.ascontiguousarray(inputs["rel_table"], f32),
        "g_final": np.asarray(inputs["g_final"], f32).reshape(1, D),
        "sinks": np.asarray(inputs["sinks"], f32).reshape(1, DEPTH * 4),
        "w_in": np.ascontiguousarray(w_in[:, :, perm]),
    }
    for k in ("g_mix", "g_group", "g_cross", "g_mem", "g_mlp", "w_out", "w_xq", "w_xkv", "w_xo", "w_up", "w_down"):
        shared[k] = np.ascontiguousarray(inputs[k], f32)
    for k, src in (("gT_cross", "g_cross"), ("gT_mlp", "g_mlp")):
        shared[k] = np.ascontiguousarray(np.asarray(inputs[src], f32).reshape(DEPTH, 8, 128).transpose(0, 2, 1))
    shared.update(_host_consts())
    x = np.asarray(inputs["x"], f32)
    mem = np.asarray(inputs["mem"], f32)
    maps = []
    for c in range(n_cores):
        m = dict(shared)
        m["x"] = np.ascontiguousarray(x[c * n_seq:(c + 1) * n_seq])
        m["mem"] = np.ascontiguousarray(mem[c * n_seq:(c + 1) * n_seq])
        maps.append(m)
    return maps


def kernel(**inputs):
    B = inputs["x"].shape[0]
    n_seq = B // N_CORES
    nc = build_program(n_seq)
    maps = make_in_maps(inputs, N_CORES, n_seq)
    res = run_bass_kernel_spmd(nc, maps, core_ids=list(range(N_CORES)))
    return np.concatenate([np.asarray(r["y"], np.float32) for r in res.results], axis=0)
```
